# Optimizing a Trainium2 kernel written in Bass

```python
import math
import jax, jax.numpy as jnp
from jax import lax
import numpy as np

D_MODEL = 1024
BATCH = 4
SEQ = 4096
DEPTH = 1

N_HEADS_A = 8
HEAD_DIM_A = 64
QK_WIDTH = N_HEADS_A * 2 * HEAD_DIM_A
V_WIDTH = N_HEADS_A * 2 * HEAD_DIM_A
Q_BLOCK = 128
CONV_WIDTH = 1024
CONV_TAPS = 31
REL_BUCKETS = 32
REL_MAX_DIST = 128
N_GROUPS = 4
EXPERTS_PER_GROUP = 8
N_EXPERTS = N_GROUPS * EXPERTS_PER_GROUP
TOP_K = 2
D_EXPERT = 512
ROW_BLOCK = 128
IN_COLS = QK_WIDTH + QK_WIDTH + V_WIDTH + 2 * CONV_WIDTH + 2 * D_MODEL
SPLITS = (QK_WIDTH, 2 * QK_WIDTH, 2 * QK_WIDTH + V_WIDTH, 2 * QK_WIDTH + V_WIDTH + 2 * CONV_WIDTH)
DN_ALPHA = (2.0 * DEPTH) ** 0.25
DN_BETA = (8.0 * DEPTH) ** -0.25
LN_EPS = 1e-5
NEG_INF = -1e30

kernel_name = "hybrid_diffattn_conformer_hmoe_deepnorm"


def layer_norm(x, g, b):
    xf = x.astype(jnp.float32)
    mu = jnp.mean(xf, axis=-1, keepdims=True)
    var = jnp.mean(jnp.square(xf - mu), axis=-1, keepdims=True)
    y = (xf - mu) * lax.rsqrt(var + LN_EPS) * g.astype(jnp.float32) + b.astype(jnp.float32)
    return y.astype(x.dtype)


def rms_norm(x, g):
    xf = x.astype(jnp.float32)
    y = xf * lax.rsqrt(jnp.mean(jnp.square(xf), axis=-1, keepdims=True) + LN_EPS) * g.astype(jnp.float32)
    return y.astype(x.dtype)


def rel_bucket(dist):
    max_exact = REL_BUCKETS // 2
    d = jnp.maximum(dist, 1).astype(jnp.float32)
    large = max_exact + (jnp.log(d / max_exact) / math.log(REL_MAX_DIST / max_exact)
                         * (REL_BUCKETS - max_exact)).astype(jnp.int32)
    large = jnp.minimum(large, REL_BUCKETS - 1)
    return jnp.where(dist < max_exact, dist, large)


def diff_attention(q, k, v, rel_table, lam):
    B, S = q.shape[0], q.shape[1]
    n_blk = S // Q_BLOCK
    scale = HEAD_DIM_A ** -0.5
    k_pos = jnp.arange(S, dtype=jnp.int32)
    q_blocks = (q * scale).reshape(B, n_blk, Q_BLOCK, N_HEADS_A, 2, HEAD_DIM_A).transpose(1, 0, 2, 3, 4, 5)
    starts = jnp.arange(n_blk, dtype=jnp.int32) * Q_BLOCK

    def one_block(args):
        q_blk, start = args
        q_pos = start + jnp.arange(Q_BLOCK, dtype=jnp.int32)
        rel = q_pos[:, None] - k_pos[None, :]
        bias = rel_table[rel_bucket(jnp.maximum(rel, 0))]
        bias = bias.transpose(2, 0, 1).astype(jnp.float32)
        logits = jnp.einsum('bqhmd,bkhmd->bhmqk', q_blk, k).astype(jnp.float32) + bias[:, None]
        logits = jnp.where(rel >= 0, logits, NEG_INF)
        p = jax.nn.softmax(logits, axis=-1)
        a = p[:, :, 0] - lam * p[:, :, 1]
        return jnp.einsum('bhqk,bkhe->bqhe', a.astype(v.dtype), v)

    out = lax.map(one_block, (q_blocks, starts))
    return out.transpose(1, 0, 2, 3, 4).reshape(B, S, N_HEADS_A, 2 * HEAD_DIM_A)


def conformer_conv(u, w_dw, b_dw, g_ln, b_ln, w_pw, b_pw):
    a, gate = jnp.split(u, 2, axis=-1)
    h = a * jax.nn.sigmoid(gate)
    h = lax.conv_general_dilated(h, w_dw[:, None, :], window_strides=(1,),
                                 padding=[(CONV_TAPS - 1, 0)],
                                 dimension_numbers=('NWC', 'WIO', 'NWC'),
                                 feature_group_count=CONV_WIDTH) + b_dw
    h = jax.nn.silu(layer_norm(h, g_ln, b_ln))
    return h @ w_pw + b_pw


def hier_moe(x, w_rg, b_rg, w_re, b_re, w_gate, w_up, w_down):
    B, S, D = x.shape
    T = B * S
    A = T * TOP_K
    xf = x.reshape(T, D)
    g_logits = (xf @ w_rg + b_rg).astype(jnp.float32)
    g_prob = jax.nn.softmax(g_logits, axis=-1)
    _, g_idx = lax.top_k(g_logits, 1)
    p_g = jnp.take_along_axis(g_prob, g_idx, axis=-1)
    e_all = (xf @ w_re + b_re).reshape(T, N_GROUPS, EXPERTS_PER_GROUP)
    e_logits = jnp.take_along_axis(e_all, g_idx[:, :, None], axis=1)[:, 0].astype(jnp.float32)
    top_v, top_i = lax.top_k(e_logits, TOP_K)
    gate = p_g * jax.nn.softmax(top_v, axis=-1)
    expert = g_idx * EXPERTS_PER_GROUP + top_i

    flat_e = expert.reshape(A)
    flat_tok = jnp.repeat(jnp.arange(T, dtype=jnp.int32), TOP_K)
    flat_w = gate.reshape(A)
    order = jnp.argsort(flat_e)
    sorted_e = flat_e[order]
    counts = jnp.zeros((N_EXPERTS,), jnp.int32).at[flat_e].add(1)
    starts = jnp.cumsum(counts) - counts
    padded = (counts + ROW_BLOCK - 1) // ROW_BLOCK * ROW_BLOCK
    pad_ends = jnp.cumsum(padded)
    pad_starts = pad_ends - padded
    dest = pad_starts[sorted_e] + (jnp.arange(A, dtype=jnp.int32) - starts[sorted_e])
    P = A + N_EXPERTS * ROW_BLOCK
    row_tok = jnp.zeros((P,), jnp.int32).at[dest].set(flat_tok[order])
    row_w = jnp.zeros((P,), jnp.float32).at[dest].set(flat_w[order])
    n_blocks = P // ROW_BLOCK
    blk_start = jnp.arange(n_blocks, dtype=jnp.int32) * ROW_BLOCK
    blk_e = jnp.minimum(jnp.sum(blk_start[:, None] >= pad_ends[None, :], axis=1), N_EXPERTS - 1)
    xs = xf[row_tok].reshape(n_blocks, ROW_BLOCK, D)

    def run_block(args):
        xb, e = args
        h = jax.nn.silu(xb @ w_gate[e]) * (xb @ w_up[e])
        return h @ w_down[e]

    ys = lax.map(run_block, (xs, blk_e)).reshape(P, D)
    out = jax.ops.segment_sum(ys * row_w[:, None].astype(ys.dtype), row_tok, num_segments=T)
    return out.reshape(B, S, D)


def setup_inputs(seed: int = 0) -> dict:
    key = jax.random.key(seed)
    ks = jax.random.split(key, 24)
    n = lambda k, shape, s: jax.random.normal(k, shape, jnp.float32) * s
    L, D, C = DEPTH, D_MODEL, CONV_WIDTH
    return {
        "x": n(ks[0], (BATCH, SEQ, D), 1.0),
        "w_in": n(ks[1], (L, D, IN_COLS), D ** -0.5),
        "b_in": n(ks[2], (L, IN_COLS), 0.02),
        "diff_lambda": n(ks[3], (L, 4, HEAD_DIM_A), 0.1),
        "head_norm_g": 1.0 + n(ks[4], (L, 2 * HEAD_DIM_A), 0.02),
        "w_o_attn": n(ks[5], (L, V_WIDTH, D), V_WIDTH ** -0.5 * DN_BETA),
        "rel_bias": n(ks[6], (REL_BUCKETS, N_HEADS_A), 0.5),
        "conv_w": n(ks[7], (L, CONV_TAPS, C), CONV_TAPS ** -0.5),
        "conv_b": n(ks[8], (L, C), 0.02),
        "conv_ln_g": 1.0 + n(ks[9], (L, C), 0.02),
        "conv_ln_b": n(ks[10], (L, C), 0.02),
        "w_conv_out": n(ks[11], (L, C, D), C ** -0.5 * DN_BETA),
        "b_conv_out": n(ks[12], (L, D), 0.02),
        "w_out": n(ks[13], (L, D, D), D ** -0.5 * DN_BETA),
        "ln1_g": 1.0 + n(ks[14], (L, D), 0.02),
        "ln1_b": n(ks[15], (L, D), 0.02),
        "router_g_w": n(ks[16], (L, D, N_GROUPS), D ** -0.5),
        "router_g_b": n(ks[17], (L, N_GROUPS), 0.01),
        "router_e_w": n(ks[18], (L, D, N_EXPERTS), D ** -0.5),
        "router_e_b": n(ks[19], (L, N_EXPERTS), 0.01),
        "expert_w_gate": n(ks[20], (L, N_EXPERTS, D, D_EXPERT), D ** -0.5),
        "expert_w_up": n(ks[21], (L, N_EXPERTS, D, D_EXPERT), D ** -0.5),
        "expert_w_down": n(ks[22], (L, N_EXPERTS, D_EXPERT, D), D_EXPERT ** -0.5 * DN_BETA),
        "ln2_g": 1.0 + n(ks[23], (L, D), 0.02),
        "ln2_b": n(jax.random.fold_in(key, 99), (L, D), 0.02),
    }


def reference(x, w_in, b_in, diff_lambda, head_norm_g, w_o_attn, rel_bias, conv_w, conv_b,
              conv_ln_g, conv_ln_b, w_conv_out, b_conv_out, w_out, ln1_g, ln1_b,
              router_g_w, router_g_b, router_e_w, router_e_b, expert_w_gate, expert_w_up,
              expert_w_down, ln2_g, ln2_b):
    B, S, D = x.shape
    for li in range(DEPTH):
        lam_init = 0.8 - 0.6 * math.exp(-0.3 * li)
        proj = x @ w_in[li] + b_in[li]
        q, k, v, u, gates = jnp.split(proj, SPLITS, axis=-1)
        q = q.reshape(B, S, N_HEADS_A, 2, HEAD_DIM_A)
        k = k.reshape(B, S, N_HEADS_A, 2, HEAD_DIM_A)
        v = v.reshape(B, S, N_HEADS_A, 2 * HEAD_DIM_A)
        lv = diff_lambda[li].astype(jnp.float32)
        lam = jnp.exp(jnp.sum(lv[0] * lv[1])) - jnp.exp(jnp.sum(lv[2] * lv[3])) + lam_init
        o = diff_attention(q, k, v, rel_bias, lam)
        o = rms_norm(o, head_norm_g[li]) * (1.0 - lam_init)
        y_a = o.reshape(B, S, V_WIDTH) @ w_o_attn[li]
        y_b = conformer_conv(u, conv_w[li], conv_b[li], conv_ln_g[li], conv_ln_b[li],
                             w_conv_out[li], b_conv_out[li])
        g_a, g_b = jnp.split(jax.nn.sigmoid(gates), 2, axis=-1)
        mixed = (g_a * y_a + g_b * y_b) @ w_out[li]
        x = layer_norm(DN_ALPHA * x + mixed, ln1_g[li], ln1_b[li])
        ffn = hier_moe(x, router_g_w[li], router_g_b[li], router_e_w[li], router_e_b[li],
                       expert_w_gate[li], expert_w_up[li], expert_w_down[li])
        x = layer_norm(DN_ALPHA * x + ffn, ln2_g[li], ln2_b[li])
    return x
```

```python
import contextlib
import math
import numpy as np
import concourse.bass as bass
import concourse.mybir as mybir
from concourse.bass_utils import run_bass_kernel_spmd

F32 = mybir.dt.float32
BF16 = mybir.dt.bfloat16
I32 = mybir.dt.int32
AF = mybir.ActivationFunctionType
ALU = mybir.AluOpType
AX = mybir.AxisListType

D = 1024
S = 4096
NBLK = 16
TOWN = 2048
H = 8
CAP = 256
NE = 32
NSLOT = NE * CAP
ALPHA = 2.0 ** 0.25
LN_EPS = 1e-5
LAM_INIT = 0.8 - 0.6 * math.exp(0.0)
BIG = 1.0e4

ENGINES = ("pe", "act", "dve", "pool", "sp")


def _region(ap):
    t = ap.tensor
    name = t.name
    dims = list(ap.ap)
    esz = mybir.dt.size(ap.dtype)
    if str(ap.space) == "DRAM":
        lo = ap.offset * esz
        hi = lo + (sum((c - 1) * abs(s) for s, c in dims) + 1) * esz
        return (name, 0, 1, lo, hi)
    pstep = dims[0][0]
    plo = ap.start_partition()
    phi = plo + ap.partition_size()
    off = ap.offset % pstep if pstep else ap.offset
    flo = off * esz
    fhi = flo + (sum((c - 1) * abs(s) for s, c in dims[1:]) + 1) * esz
    return (name, plo, phi, flo, fhi)


class Op:
    __slots__ = ("eng", "fn", "dma_key", "deps", "dma_need", "sig", "rank", "is_dma")

    def __init__(self, eng, fn, dma_key):
        self.eng = eng
        self.fn = fn
        self.dma_key = dma_key
        self.is_dma = dma_key is not None
        self.deps = []
        self.dma_need = {}
        self.sig = False
        self.rank = None


class Prog:
    def __init__(self, glob):
        self.g = glob
        self.ops = {e: [] for e in ENGINES}
        self.hist = {}

    def add(self, eng, fn, reads=(), writes=(), dma_key=None):
        rr = [a if isinstance(a, tuple) else _region(a) for a in reads]
        ww = [a if isinstance(a, tuple) else _region(a) for a in writes]
        op = Op(eng, fn, dma_key)
        self.ops[eng].append(op)
        g = self.g
        deps = {}
        for (name, plo, phi, flo, fhi) in rr:
            for rec in self.hist.setdefault(name, []):
                if rec[5] and rec[0] < phi and plo < rec[1] and rec[2] < fhi and flo < rec[3]:
                    deps[id(rec[4])] = rec[4]
        for (name, plo, phi, flo, fhi) in ww:
            keep = []
            for rec in self.hist.setdefault(name, []):
                if rec[0] < phi and plo < rec[1] and rec[2] < fhi and flo < rec[3]:
                    deps[id(rec[4])] = rec[4]
                    if plo <= rec[0] and rec[1] <= phi and flo <= rec[2] and rec[3] <= fhi:
                        continue
                keep.append(rec)
            self.hist[name] = keep
        for (name, plo, phi, flo, fhi) in rr:
            h = self.hist[name]
            if not op.is_dma:
                h[:] = [rec for rec in h if not ((not rec[5]) and rec[4].eng == eng and not rec[4].is_dma
                                                   and rec[0] == plo and rec[1] == phi
                                                   and rec[2] == flo and rec[3] == fhi)]
            h.append([plo, phi, flo, fhi, op, False])
        for (name, plo, phi, flo, fhi) in ww:
            self.hist[name].append([plo, phi, flo, fhi, op, True])
        deps.pop(id(op), None)
        for d in deps.values():
            if d.is_dma:
                op.dma_need[d.dma_key] = 16 * g.dma_count[d.dma_key]
        if dma_key is not None:
            g.dma_count[dma_key] = g.dma_count.get(dma_key, 0) + 1
        for d in deps.values():
            if d.is_dma:
                continue
            elif d.eng == "pe" and eng == "pe":
                continue
            else:
                op.deps.append(d)
                d.sig = True
        return op


class Glob:
    def __init__(self, nc, st):
        self.nc = nc
        self.dma_count = {}
        self.dma_sem = {}
        self.st = st
        self.esem = {e: st.enter_context(nc.semaphore(f"s_{e}")) for e in ENGINES}
        self.bar = st.enter_context(nc.semaphore("s_bar"))
        self.rank = {e: 0 for e in ENGINES}
        self.waited = {e: {} for e in ENGINES}
        self.nbar = 0

    def dsem(self, key):
        if key not in self.dma_sem:
            self.dma_sem[key] = self.st.enter_context(self.nc.semaphore(f"d_{len(self.dma_sem)}"))
        return self.dma_sem[key]


def run_phase(g, prog):
    nc = g.nc
    for e in ENGINES:
        for op in reversed(prog.ops[e]):
            if not op.is_dma:
                op.sig = True
                break
    for e in ENGINES:
        for op in prog.ops[e]:
            if op.sig and not op.is_dma:
                g.rank[e] += 1
                op.rank = g.rank[e]
    g.nbar += 1
    nbar = g.nbar
    for k in g.dma_count:
        g.dsem(k)

    def body(e, eng):
        waited = g.waited[e]
        last_rank = 0
        for op in prog.ops[e]:
            need = {}
            for d in op.deps:
                key = ("e", d.eng)
                if waited.get(key, 0) < d.rank and need.get(key, (None, 0))[1] < d.rank:
                    need[key] = (g.esem[d.eng], d.rank)
            for k, val in op.dma_need.items():
                key = ("d", k)
                if waited.get(key, 0) < val and need.get(key, (None, 0))[1] < val:
                    need[key] = (g.dsem(k), val)
            for key, (sem, val) in need.items():
                eng.wait_ge(sem, val)
                waited[key] = val
            ins = op.fn(eng)
            if op.is_dma:
                ins.then_inc(g.dsem(op.dma_key), 16)
            elif op.sig:
                ins.then_inc(g.esem[e], 1)
                last_rank = op.rank
        if last_rank and waited.get(("e", e), 0) < last_rank:
            eng.wait_ge(g.esem[e], last_rank)
            waited[("e", e)] = last_rank
        seen = set()
        for op in prog.ops[e]:
            if op.is_dma and op.dma_key not in seen:
                seen.add(op.dma_key)
                val = 16 * g.dma_count[op.dma_key]
                if waited.get(("d", op.dma_key), 0) < val:
                    eng.wait_ge(g.dsem(op.dma_key), val)
                    waited[("d", op.dma_key)] = val
        eng.sem_inc(g.bar, 1)
        eng.wait_ge(g.bar, 5 * nbar)

    with nc.Block() as block:
        @block.tensor
        def _(eng):
            body("pe", eng)

        @block.scalar
        def _(eng):
            body("act", eng)

        @block.vector
        def _(eng):
            body("dve", eng)

        @block.gpsimd
        def _(eng):
            body("pool", eng)

        @block.sync
        def _(eng):
            body("sp", eng)


def _isap(x):
    return not isinstance(x, (int, float)) and x is not None


def MM(P, out, lhsT, rhs, start=True, stop=True):
    P.add("pe", lambda e: e.matmul(out, lhsT, rhs, start=start, stop=stop), [lhsT, rhs], [out])


def TR(P, out, in_, ident):
    P.add("pe", lambda e: e.transpose(out, in_, ident), [in_, ident], [out])


def ACT(P, out, in_, func, bias=None, scale=1.0, accum_out=None):
    reads = [in_] + [a for a in (bias, scale) if _isap(a)]
    writes = [out] + ([accum_out] if accum_out is not None else [])
    kw = {}
    if bias is not None:
        kw["bias"] = bias
    if accum_out is not None:
        kw["accum_out"] = accum_out
    P.add("act", lambda e: e.activation(out=out, in_=in_, func=func, scale=scale, **kw), reads, writes)


def TT(P, out, in0, in1, op, eng="dve"):
    P.add(eng, lambda e: e.tensor_tensor(out=out, in0=in0, in1=in1, op=op), [in0, in1], [out])


def TS(P, out, in0, s1, op0, s2=None, op1=None, eng="dve"):
    reads = [in0] + [a for a in (s1, s2) if _isap(a)]
    if op1 is None:
        P.add(eng, lambda e: e.tensor_scalar(out=out, in0=in0, scalar1=s1, scalar2=None, op0=op0), reads, [out])
    else:
        P.add(eng, lambda e: e.tensor_scalar(out=out, in0=in0, scalar1=s1, scalar2=s2, op0=op0, op1=op1),
              reads, [out])


def STT(P, out, in0, scalar, in1, op0, op1):
    reads = [in0, in1] + ([scalar] if _isap(scalar) else [])
    P.add("dve", lambda e: e.scalar_tensor_tensor(out=out, in0=in0, scalar=scalar, in1=in1, op0=op0, op1=op1),
          reads, [out])


def CP(P, out, in_, eng="dve"):
    if eng == "act":
        P.add("act", lambda e: e.activation(out=out, in_=in_, func=AF.Copy), [in_], [out])
    else:
        P.add(eng, lambda e: e.tensor_copy(out=out, in_=in_), [in_], [out])


def RED(P, out, in_, op):
    P.add("dve", lambda e: e.tensor_reduce(out=out, in_=in_, op=op, axis=AX.X), [in_], [out])


def RCP(P, out, in_):
    P.add("dve", lambda e: e.reciprocal(out=out, in_=in_), [in_], [out])


def MSET(P, ap, val, eng="dve"):
    P.add(eng, lambda e: e.memset(ap, val), [], [ap])


def DMA(P, out, in_, key, eng="sp"):
    P.add(eng, lambda e: e.dma_start(out=out, in_=in_), [in_], [out], dma_key=key)


def build_nc(debug=False, stop_after=99):
    nc = bass.Bass("TRN2", target_bir_lowering=False)

    def inp(name, shape, dt=F32):
        return nc.dram_tensor(name, list(shape), dt, kind="ExternalInput").ap()

    def scr(name, shape, dt):
        return nc.dram_tensor(name, list(shape), dt, kind=("ExternalOutput" if debug else "Internal")).ap()

    xT = inp("xT", [D, S])
    xhT = inp("xhT", [D, 512])
    xtok = inp("xtok", [TOWN, D])
    w_in = inp("w_in", [D, 7168])
    bcol_d = inp("bcol", [128, 56])
    bv_d = inp("bv_bc", [128, 1024])
    hm_d = inp("hm", [128, 1])
    lam_d = inp("lam_bc", [128, 256])
    hng_d = inp("hng_bc", [128, 128])
    bias_d = inp("biasT", [128, 8 * 3 * 128])
    relc_d = inp("relc", [128, 8])
    ident_d = inp("ident", [128, 128])
    tri_d = inp("tri", [128, 128])
    ecoff_d = inp("ecoff", [128, 32])
    convw_d = inp("convw", [128, 8 * 31])
    cvec_d = inp("cvec", [128, 4 * 8])
    w_oa = inp("w_o_attn", [D, D])
    w_co = inp("w_conv_out", [D, D])
    w_out = inp("w_out", [D, D])
    lnp_d = inp("lnp", [128, 4 * 1024])
    wr_d = inp("w_r", [D, 36])
    br_d = inp("b_r", [128, 36])
    wg_d = inp("e_wg", [NE, D, 512])
    wu_d = inp("e_wu", [NE, D, 512])
    wd_d = inp("e_wd", [NE, 512, D])
    y = nc.dram_tensor("y", [TOWN, D], F32, kind="ExternalOutput").ap()

    KT_s = scr("KT_s", [H, 2, 128, S], BF16)
    QT_s = scr("QT_s", [H, 128, TOWN], BF16)
    VA_s = scr("VA_s", [32, 128, H * 256], BF16)
    G_s = scr("G_s", [16, 128, TOWN], BF16)
    H_s = scr("H_s", [8, 128, 16, 160], F32)
    X1F_s = scr("X1F_s", [TOWN, D], F32)
    XS_s = scr("XS_s", [NSLOT, D], BF16)
    YS_s = scr("YS_s", [NSLOT, D], F32)
    DBG_O = scr("DBG_O", [TOWN, D], F32) if debug else None
    DBG_C = scr("DBG_C", [D, TOWN], F32) if debug else None
    DBG_M = scr("DBG_M", [D, TOWN], F32) if debug else None

    with contextlib.ExitStack() as top:
        g = Glob(nc, top)
        ps = [top.enter_context(nc.psum_tensor(f"ps{i}", [128, 512], F32)) for i in range(8)]
        sb = lambda st, name, shape, dt: st.enter_context(nc.sbuf_tensor("s_" + name, list(shape), dt))
        ident = sb(top, "ident", [128, 128], BF16)
        cvec = sb(top, "cvec", [128, 32], F32)
        epsc = sb(top, "epsc", [128, 1], F32)

        if stop_after >= 1:
            with contextlib.ExitStack() as st:
                P = Prog(g)
                W = sb(st, "W", [128, 8, 7168], BF16)
                xt = [sb(st, f"xt{i}", [128, 8, 512], BF16) for i in range(2)]
                bcol = sb(st, "bcol", [128, 56], F32)
                bv = sb(st, "bv", [128, 1024], F32)
                hm = sb(st, "hm", [128, 1], F32)
                KA = [sb(st, f"KA{i}", [128, 512], BF16) for i in range(2)]
                KB = [sb(st, f"KB{i}", [128, 512], BF16) for i in range(2)]
                Qs = [sb(st, f"Qs{i}", [128, 512], BF16) for i in range(2)]
                Gs = [sb(st, f"Gs{i}", [128, 512], BF16) for i in range(3)]
                Hs = [sb(st, f"Hs{i}", [128, 512], F32) for i in range(2)]
                Sg = [sb(st, f"Sg{i}", [128, 512], F32) for i in range(2)]
                Vs = [sb(st, f"Vs{i}", [128, 8, 256], BF16) for i in range(2)]

                DMA(P, bcol[:], bcol_d, "bcol")
                DMA(P, bv[:], bv_d, "bv")
                DMA(P, hm[:], hm_d, "hm")
                DMA(P, cvec[:], cvec_d, "cvec")
                DMA(P, ident[:], ident_d, "ident", eng="pool")
                MSET(P, epsc[:], LN_EPS)
                for i in range(2):
                    MSET(P, KA[i][:], 0.0)
                    MSET(P, KB[i][:], 0.0)
                    MSET(P, Vs[i][:], 1.0)
                w_v = w_in.rearrange("(c p) n -> p c n", p=128)
                xT_v = xT.rearrange("(c p) t -> p c t", p=128)
                xhT_v = xhT.rearrange("(c p) t -> p c t", p=128)
                DMA(P, xt[0][:], xT_v[:, :, 0:512], "xt0", eng="pool")
                for grp in (1, 2, 0, 3, 4, 5, 6):
                    DMA(P, W[:, :, grp * 1024:(grp + 1) * 1024], w_v[:, :, grp * 1024:(grp + 1) * 1024],
                        f"W{grp}", eng="pool")
                bank = [0]

                def nb():
                    b = ps[bank[0] % 8]
                    bank[0] += 1
                    return b

                cnt = {"k": 0, "q": 0, "g": 0, "h": 0, "v": 0}
                import os
                PARTS = os.environ.get('PH1_PARTS', 'kvqgh')
                for t in range(9):
                    if t >= int(os.environ.get('PH1_T', '9')):
                        break
                    X = xt[t % 2]
                    if t + 1 < 9:
                        src = xT_v[:, :, (t + 1) * 512:(t + 2) * 512] if t + 1 < 8 else xhT_v
                        DMA(P, xt[(t + 1) % 2][:], src, f"xt{(t + 1) % 2}", eng="pool")

                    def proj(ch):
                        b = nb()
                        for dc in range(8):
                            MM(P, b[:], W[:, dc, ch * 128:(ch + 1) * 128], X[:, dc, :], start=(dc == 0), stop=(dc == 7))
                        return b

                    if t < 8 and 'k' in PARTS:
                        for hh in range(8):
                            b = proj(8 + hh)
                            ka, kb = KA[cnt["k"] % 2], KB[cnt["k"] % 2]
                            cnt["k"] += 1
                            TS(P, ka[0:64, :], b[0:64, :], bcol[0:64, 8 + hh:9 + hh], ALU.add)
                            ACT(P, kb[64:128, :], b[64:128, :], AF.Identity, bias=bcol[64:128, 8 + hh:9 + hh])
                            DMA(P, KT_s[hh, 0, :, t * 512:(t + 1) * 512], ka[:], ka.name)
                            DMA(P, KT_s[hh, 1, :, t * 512:(t + 1) * 512], kb[:], kb.name)
                        for blk in range(4 if 'v' in PARTS else 0):
                            vs = Vs[cnt["v"] % 2]
                            cnt["v"] += 1
                            for hf in range(2):
                                b = nb()
                                for dc in range(8):
                                    MM(P, b[:], X[:, dc, blk * 128:(blk + 1) * 128],
                                       W[:, dc, 2048 + hf * 512:2048 + (hf + 1) * 512], start=(dc == 0), stop=(dc == 7))
                                TT(P, vs[:, hf * 4:(hf + 1) * 4, 0:128], b[:].rearrange("p (h e) -> p h e", e=128),
                                   bv[:, hf * 512:(hf + 1) * 512].rearrange("p (h e) -> p h e", e=128), ALU.add)
                            if os.environ.get('VDMA', '1') == '1':
                                DMA(P, VA_s[t * 4 + blk], vs[:].rearrange("p h e -> p (h e)"), vs.name)
                    if t < 4 and 'q' in PARTS:
                        for hh in range(8):
                            b = proj(hh)
                            qs = Qs[cnt["q"] % 2]
                            cnt["q"] += 1
                            TS(P, qs[:], b[:], bcol[:, hh:hh + 1], ALU.add, 0.125, ALU.mult)
                            DMA(P, QT_s[hh, :, t * 512:(t + 1) * 512], qs[:], qs.name)
                        for c in range(16 if 'g' in PARTS else 0):
                            b = proj(40 + c)
                            gs = Gs[cnt["g"] % 3]
                            cnt["g"] += 1
                            ACT(P, gs[:], b[:], AF.Sigmoid, bias=bcol[:, 40 + c:41 + c])
                            DMA(P, G_s[c, :, t * 512:(t + 1) * 512], gs[:], gs.name)
                    if (t < 4 or t == 8) and 'h' in PARTS:
                        for c in range(8):
                            ba = proj(24 + c)
                            bg = proj(32 + c)
                            sg = Sg[cnt["h"] % 2]
                            hs = Hs[cnt["h"] % 2]
                            cnt["h"] += 1
                            ACT(P, sg[:], bg[:], AF.Sigmoid, bias=bcol[:, 32 + c:33 + c])
                            STT(P, hs[:], ba[:], bcol[:, 24 + c:25 + c], sg[:], ALU.add, ALU.mult)
                            if t == 8:
                                TS(P, hs[:, 0:32], hs[:, 0:32], hm[:, 0:1], ALU.mult)
                                DMA(P, H_s[c, :, :, 0:32], hs[:].rearrange("p (b e) -> p b e", e=32), hs.name)
                            else:
                                DMA(P, H_s[c, :, t * 4:(t + 1) * 4, 32:160],
                                    hs[:].rearrange("p (b e) -> p b e", e=128), hs.name)
                run_phase(g, P)

        conv_out = sb(top, "conv_out", [128, 8, TOWN], BF16)
        O_all = sb(top, "O_all", [128, NBLK, D], BF16)
        if stop_after >= 2:
            with contextlib.ExitStack() as st:
                P = Prog(g)
                KT = [sb(st, f"KT{i}", [128, 2, S], BF16) for i in range(2)]
                QT = [sb(st, f"QT{i}", [128, TOWN], BF16) for i in range(2)]
                VA = [sb(st, f"VA{i}", [128, 32, 256], BF16) for i in range(2)]
                PT = [sb(st, f"PT{i}", [128, 512], BF16) for i in range(3)]
                bias_f = sb(st, "bias_f", [128, 8, 384], F32)
                bias_b = sb(st, "bias_b", [128, 8, 3, 128], BF16)
                relc = sb(st, "relc", [128, 8], F32)
                lamb = sb(st, "lamb", [128, 4, 64], F32)
                lt = sb(st, "lt", [128, 8], F32)
                gv = sb(st, "gv", [128, 128], F32)
                hb = [sb(st, f"hb{i}", [128, 16, 160], F32) for i in range(2)]
                acc = sb(st, "acc", [128, 16, 128], F32)
                convw = sb(st, "convw", [128, 8, 31], F32)
                rr_ = [sb(st, f"rr{i}", [128, 8], F32) for i in range(2)]
                o1 = [sb(st, f"o1_{i}", [128, 128], F32) for i in range(2)]
                o2 = [sb(st, f"o2_{i}", [128, 128], F32) for i in range(2)]
                junk = sb(st, "junk", [128, 128], F32)

                DMA(P, bias_f[:], bias_d.rearrange("p (h x) -> p h x", x=384), "bias_f")
                DMA(P, relc[:], relc_d, "relc")
                DMA(P, lamb[:], lam_d.rearrange("p (a b) -> p a b", b=64), "lamb")
                DMA(P, gv[:], hng_d, "gv")
                DMA(P, convw[:], convw_d.rearrange("p (c k) -> p c k", k=31), "convw")
                for h in range(8):
                    TS(P, bias_b[:, h].rearrange("p a b -> p (a b)"), bias_f[:, h, :], relc[:, h:h + 1], ALU.subtract)
                TT(P, lamb[:, 0, :], lamb[:, 0, :], lamb[:, 1, :], ALU.mult)
                TT(P, lamb[:, 2, :], lamb[:, 2, :], lamb[:, 3, :], ALU.mult)
                RED(P, lt[:, 0:1], lamb[:, 0, :], ALU.add)
                RED(P, lt[:, 1:2], lamb[:, 2, :], ALU.add)
                ACT(P, lt[:, 2:4], lt[:, 0:2], AF.Exp)
                TT(P, lt[:, 4:5], lt[:, 3:4], lt[:, 2:3], ALU.subtract)
                TS(P, lt[:, 4:5], lt[:, 4:5], -LAM_INIT, ALU.add)
                TS(P, gv[:], gv[:], 1.0 - LAM_INIT, ALU.mult)

                def load_head(h):
                    s = h % 2
                    DMA(P, KT[s][:], KT_s[h].rearrange("m p t -> p m t"), f"KT{s}", eng="pool")
                    DMA(P, QT[s][:], QT_s[h], f"QT{s}", eng="pool")
                    DMA(P, VA[s][:], VA_s[:, :, h * 256:(h + 1) * 256].rearrange("b p e -> p b e"), f"VA{s}", eng="pool")

                def load_h(c):
                    DMA(P, hb[c % 2][:], H_s[c], f"hb{c % 2}", eng="pool")

                load_head(0)
                load_h(0)
                groups = []
                for h in range(8):
                    for i in range(NBLK):
                        lst = [(j, None) for j in range(i)] + [(16 + j, None) for j in range(i - 1)]
                        if i >= 1:
                            lst.append((16 + i - 1, 2))
                        lst.append((16 + i, 1))
                        lst.append((i, 0))
                        for m in range(2):
                            chunks = [lst[a:a + 4] for a in range(0, len(lst), 4)]
                            for ci, ch in enumerate(chunks):
                                groups.append((h, i, m, ch, ci == 0, ci == len(chunks) - 1))
                conv_ops = []

                def emit_qk(n):
                    h, i, m, ch, first, last = groups[n]
                    s = h % 2
                    Sb = ps[n % 3]
                    for j, (kb, bt) in enumerate(ch):
                        MM(P, Sb[:, j * 128:(j + 1) * 128], KT[s][:, m, kb * 128:(kb + 1) * 128],
                           QT[s][:, i * 128:(i + 1) * 128], start=True, stop=(bt is None))
                        if bt is not None:
                            MM(P, Sb[:, j * 128:(j + 1) * 128], ident[:], bias_b[:, h, bt, :], start=False, stop=True)
                    w = len(ch) * 128
                    ACT(P, PT[n % 3][:, 0:w], Sb[:, 0:w], AF.Exp)

                def emit_av(n):
                    h, i, m, ch, first, last = groups[n]
                    s = h % 2
                    par = (h * NBLK + i) % 2
                    Ob = ps[3 + 2 * par + m]
                    for j, (kb, bt) in enumerate(ch):
                        MM(P, Ob[:, 0:130], PT[n % 3][:, j * 128:(j + 1) * 128], VA[s][:, kb, 0:130],
                           start=(first and j == 0), stop=(last and j == len(ch) - 1))
                    if last and m == 1:
                        combine(h, i, par)

                def combine(h, i, par):
                    Oa, Ob = ps[3 + 2 * par], ps[4 + 2 * par]
                    r = rr_[par]
                    RCP(P, r[:, 0:1], Oa[:, 128:129])
                    RCP(P, r[:, 1:2], Ob[:, 128:129])
                    TS(P, r[:, 2:3], r[:, 1:2], lt[:, 4:5], ALU.mult)
                    TS(P, o1[par][:], Oa[:, 0:128], r[:, 0:1], ALU.mult)
                    STT(P, o2[par][:], Ob[:, 0:128], r[:, 2:3], o1[par][:], ALU.mult, ALU.add)
                    ACT(P, junk[:], o2[par][:], AF.Square, accum_out=r[:, 3:4])
                    ACT(P, r[:, 4:5], r[:, 3:4], AF.Ln, bias=epsc[:, 0:1], scale=1.0 / 128.0)
                    ACT(P, r[:, 5:6], r[:, 4:5], AF.Exp, scale=-0.5)
                    STT(P, O_all[:, i, h * 128:(h + 1) * 128], o2[par][:], r[:, 5:6], gv[:], ALU.mult, ALU.mult)
                    c = h
                    for k in (2 * i, 2 * i + 1):
                        if k > 30:
                            continue
                        src = hb[c % 2][:, :, 2 + k:2 + k + 128]
                        if k == 0:
                            TS(P, acc[:], src, convw[:, c, 0:1], ALU.mult, cvec[:, c:c + 1], ALU.add)
                        elif k < 30:
                            STT(P, acc[:], src, convw[:, c, k:k + 1], acc[:], ALU.mult, ALU.add)
                        else:
                            STT(P, conv_out[:, c, :].rearrange("p (b e) -> p b e", e=128), src,
                                convw[:, c, k:k + 1], acc[:], ALU.mult, ALU.add)

                prev_h = 0
                for n in range(len(groups) + 1):
                    newhead = n < len(groups) and groups[n][0] != prev_h
                    if n < len(groups):
                        emit_qk(n)
                    if n >= 1:
                        emit_av(n - 1)
                    if n == 0 or newhead:
                        prev_h = groups[n][0]
                        if prev_h + 1 < 8:
                            load_head(prev_h + 1)
                            load_h(prev_h + 1)
                if debug:
                    for i in range(NBLK):
                        DMA(P, DBG_O[i * 128:(i + 1) * 128, :], O_all[:, i, :], "dbg0", eng="pool")
                    for c in range(8):
                        DMA(P, DBG_C[c * 128:(c + 1) * 128, :], conv_out[:, c, :], "dbg1", eng="pool")
                run_phase(g, P)

        if stop_after >= 3:
            with contextlib.ExitStack() as st:
                P = Prog(g)
                oT = sb(st, "oT", [128, 8, TOWN], BF16)
                mT = sb(st, "mT", [128, 8, TOWN], BF16)
                ones = sb(st, "ones", [128, 128], BF16)
                Wc = [sb(st, f"Wc{i}", [128, 2, 8, 128], BF16) for i in range(2)]
                Wo = sb(st, "Wo", [128, 8, D], BF16)
                lnp = sb(st, "lnp", [128, 2, D], F32)
                sq = [sb(st, f"sq{i}", [128, 512], BF16) for i in range(2)]
                mean = sb(st, "mean", [128, 512], F32)
                rstd = sb(st, "rstd", [128, 512], F32)
                t1 = [sb(st, f"t1_{i}", [128, 512], F32) for i in range(2)]
                t2 = [sb(st, f"t2_{i}", [128, 512], F32) for i in range(2)]
                gab = [sb(st, f"gab{i}", [128, 2, 512], BF16) for i in range(2)]
                xb = [sb(st, f"xb{i}", [128, D], F32) for i in range(2)]
                z = [sb(st, f"z{i}", [128, D], F32) for i in range(2)]
                bst = [sb(st, f"bst{i}", [128, 2, 6], F32) for i in range(2)]
                mv = [sb(st, f"mv{i}", [128, 4], F32) for i in range(2)]
                bank = [0]

                def nb():
                    b = ps[bank[0] % 8]
                    bank[0] += 1
                    return b

                MSET(P, ones[:], 1.0 / 1024.0)
                DMA(P, lnp[:], lnp_d[:, 0:2048].rearrange("p (a b) -> p a b", b=1024), "lnp")
                DMA(P, Wo[:], w_out.rearrange("(c p) n -> p c n", p=128), "Wo", eng="pool")

                def load_wc(c):
                    s = c % 2
                    DMA(P, Wc[s][:, 0], w_oa.rearrange("(c p) n -> p c n", p=128)[:, :, c * 128:(c + 1) * 128],
                        f"Wc{s}", eng="pool")
                    DMA(P, Wc[s][:, 1], w_co.rearrange("(c p) n -> p c n", p=128)[:, :, c * 128:(c + 1) * 128],
                        f"Wc{s}", eng="pool")

                load_wc(0)
                for i in range(NBLK):
                    for hf in range(2):
                        b = nb()
                        bb = b[:, 0:256].bitcast(BF16)
                        for hh in range(4):
                            h = hf * 4 + hh
                            TR(P, bb[:, hh * 128:(hh + 1) * 128], O_all[:, i, h * 128:(h + 1) * 128], ident[:])
                        CP(P, oT[:, hf * 4:(hf + 1) * 4, i * 128:(i + 1) * 128],
                           bb.rearrange("p (h e) -> p h e", e=128), eng=("dve" if hf else "act"))
                for t in range(4):
                    sl = slice(t * 512, (t + 1) * 512)
                    bm = nb()
                    for c in range(8):
                        MM(P, bm[:], ones[:], conv_out[:, c, sl], start=(c == 0), stop=(c == 7))
                    be = nb()
                    for c in range(8):
                        s_ = sq[c % 2]
                        TT(P, s_[:], conv_out[:, c, sl], conv_out[:, c, sl], ALU.mult)
                        MM(P, be[:], ones[:], s_[:], start=(c == 0), stop=(c == 7))
                    CP(P, mean[:], bm[:])
                    TT(P, rstd[:], mean[:], mean[:], ALU.mult)
                    TT(P, rstd[:], be[:], rstd[:], ALU.subtract)
                    ACT(P, rstd[:], rstd[:], AF.Sqrt, bias=epsc[:, 0:1])
                    RCP(P, rstd[:], rstd[:])
                    for c in range(8):
                        a_ = t1[c % 2]
                        TT(P, a_[:], conv_out[:, c, sl], mean[:], ALU.subtract)
                        TT(P, a_[:], a_[:], rstd[:], ALU.mult)
                        ACT(P, conv_out[:, c, sl], a_[:], AF.Silu, bias=cvec[:, 16 + c:17 + c], scale=cvec[:, 8 + c:9 + c])
                wv = w_in.rearrange("(c p) n -> p c n", p=128)
                for c in range(8):
                    if c + 1 < 8:
                        load_wc(c + 1)
                    for t in range(4):
                        sl = slice(t * 512, (t + 1) * 512)
                        gb_ = gab[(c * 4 + t) % 2]
                        DMA(P, gb_[:, 0, :], G_s[c, :, sl], gb_.name, eng="pool")
                        DMA(P, gb_[:, 1, :], G_s[8 + c, :, sl], gb_.name, eng="pool")
                        ba = nb()
                        for vc in range(8):
                            MM(P, ba[:], Wc[c % 2][:, 0, vc, :], oT[:, vc, sl], start=(vc == 0), stop=(vc == 7))
                        bb = nb()
                        for vc in range(8):
                            MM(P, bb[:], Wc[c % 2][:, 1, vc, :], conv_out[:, vc, sl], start=(vc == 0), stop=(vc == 7))
                        a_ = t1[t % 2]
                        b_ = t2[t % 2]
                        TT(P, a_[:], ba[:], gb_[:, 0, :], ALU.mult)
                        STT(P, b_[:], bb[:], cvec[:, 24 + c:25 + c], gb_[:, 1, :], ALU.add, ALU.mult)
                        TT(P, mT[:, c, sl], a_[:], b_[:], ALU.add, eng="pool")
                if debug:
                    for c in range(8):
                        DMA(P, DBG_M[c * 128:(c + 1) * 128, :], mT[:, c, :], "dbg2", eng="pool")
                for i in range(NBLK):
                    X = xb[i % 2]
                    Z = z[i % 2]
                    DMA(P, X[:], xtok[i * 128:(i + 1) * 128, :], X.name)
                    for hf in range(2):
                        b = nb()
                        for dc in range(8):
                            MM(P, b[:], mT[:, dc, i * 128:(i + 1) * 128], Wo[:, dc, hf * 512:(hf + 1) * 512],
                               start=(dc == 0), stop=(dc == 7))
                        STT(P, Z[:, hf * 512:(hf + 1) * 512], X[:, hf * 512:(hf + 1) * 512], ALPHA, b[:], ALU.mult, ALU.add)
                        P.add("dve", (lambda o, a: (lambda e: e.bn_stats(out=o, in_=a)))(bst[i % 2][:, hf, :], Z[:, hf * 512:(hf + 1) * 512]),
                              [Z[:, hf * 512:(hf + 1) * 512]], [bst[i % 2][:, hf, :]])
                    M = mv[i % 2]
                    P.add("dve", (lambda o, a: (lambda e: e.bn_aggr(out=o, in_=a)))(M[:, 0:2], bst[i % 2][:]),
                          [bst[i % 2][:]], [M[:, 0:2]])
                    ACT(P, M[:, 2:3], M[:, 1:2], AF.Sqrt, bias=epsc[:, 0:1])
                    RCP(P, M[:, 3:4], M[:, 2:3])
                    TS(P, Z[:], Z[:], M[:, 0:1], ALU.subtract, M[:, 3:4], ALU.mult)
                    TT(P, Z[:], Z[:], lnp[:, 0, :], ALU.mult, eng="pool")
                    TT(P, Z[:], Z[:], lnp[:, 1, :], ALU.add)
                    CP(P, O_all[:, i, :], Z[:], eng="act")
                    DMA(P, X1F_s[i * 128:(i + 1) * 128, :], Z[:], Z.name)
                run_phase(g, P)

        if stop_after >= 4:
            x1b = O_all
            with contextlib.ExitStack() as st:
                P = Prog(g)
                x1T = conv_out
                wr = sb(st, "wr", [128, 8, 36], BF16)
                brt = sb(st, "brt", [128, 36], F32)
                tri = sb(st, "tri", [128, 128], BF16)
                onesb = sb(st, "onesb", [128, 128], BF16)
                ecoff = sb(st, "ecoff", [128, 32], F32)
                LG = sb(st, "LG", [128, 16, 36], F32)
                EM = sb(st, "EM", [128, 16, 32], F32)
                EM2 = sb(st, "EM2", [128, 16, 32], F32)
                mk1 = sb(st, "mk1", [128, 16, 32], F32)
                mk2 = sb(st, "mk2", [128, 16, 32], F32)
                slot = sb(st, "slot", [128, 16, 32], F32)
                tmp3 = sb(st, "tmp3", [128, 16, 32], F32)
                selb = sb(st, "selb", [128, 16, 32], BF16)
                G4 = sb(st, "G4", [128, 16, 4], F32)
                gm = sb(st, "gm", [128, 16, 4], F32)
                sm = sb(st, "sm", [128, 12, 16], F32)
                di = sb(st, "di", [128, 2, 16], I32)
                EW = [sb(st, f"EW{i}", [128, 3, 8, 512], BF16) for i in range(2)]
                xg = [sb(st, f"xg{i}", [128, 2, D], BF16) for i in range(2)]
                xeT = [sb(st, f"xeT{i}", [128, 8, CAP], BF16) for i in range(2)]
                hT = [sb(st, f"hT{i}", [128, 4, CAP], BF16) for i in range(2)]
                sg = [sb(st, f"sgm{i}", [128, CAP], F32) for i in range(2)]
                yst = [sb(st, f"yst{i}", [128, D], F32) for i in range(2)]
                lnp = sb(st, "lnp2", [128, 2, D], F32)
                y1 = [sb(st, f"y1_{i}", [128, D], F32) for i in range(2)]
                y2 = [sb(st, f"y2_{i}", [128, D], F32) for i in range(2)]
                xf = [sb(st, f"xf{i}", [128, D], F32) for i in range(2)]
                bst = [sb(st, f"bs2{i}", [128, 2, 6], F32) for i in range(2)]
                mv = [sb(st, f"mv2{i}", [128, 4], F32) for i in range(2)]
                bank = [0]

                def nb():
                    b = ps[bank[0] % 8]
                    bank[0] += 1
                    return b

                def load_e(e):
                    s = e % 2
                    DMA(P, EW[s][:, 0], wg_d[e].rearrange("(c p) n -> p c n", p=128), f"EWa{s}", eng="pool")
                    DMA(P, EW[s][:, 1], wu_d[e].rearrange("(c p) n -> p c n", p=128), f"EWb{s}", eng="pool")
                    DMA(P, EW[s][:, 2].rearrange("p (a b) n -> p a (b n)", b=2),
                        wd_d[e].rearrange("(c p) n -> p c n", p=128), f"EWc{s}", eng="pool")

                DMA(P, wr[:], wr_d.rearrange("(c p) n -> p c n", p=128), "wr", eng="pool")
                DMA(P, tri[:], tri_d, "tri", eng="pool")
                DMA(P, brt[:], br_d, "brt")
                DMA(P, ecoff[:], ecoff_d, "ecoff")
                DMA(P, lnp[:], lnp_d[:, 2048:4096].rearrange("p (a b) -> p a b", b=1024), "lnp2")
                MSET(P, onesb[:], 1.0)
                zt = sb(st, "zt", [128, 4096], BF16)
                MSET(P, zt[:], 0.0, eng="pool")
                for zi in range(16):
                    DMA(P, XS_s[zi * 512:(zi + 1) * 512, :].rearrange("(p a) d -> p (a d)", p=128), zt[:], "zt")
                load_e(0)
                load_e(1)
                for i in range(NBLK):
                    for hf in range(2):
                        b = nb()
                        bb = b[:, 0:256].bitcast(BF16)
                        for hh in range(4):
                            dc = hf * 4 + hh
                            TR(P, bb[:, hh * 128:(hh + 1) * 128], x1b[:, i, dc * 128:(dc + 1) * 128], ident[:])
                        CP(P, x1T[:, hf * 4:(hf + 1) * 4, i * 128:(i + 1) * 128],
                           bb.rearrange("p (h e) -> p h e", e=128), eng=("dve" if hf else "act"))
                for half in range(2):
                    b = nb()
                    for ii in range(8):
                        i = half * 8 + ii
                        for dc in range(8):
                            MM(P, b[:, ii * 36:(ii + 1) * 36], x1T[:, dc, i * 128:(i + 1) * 128], wr[:, dc, :],
                               start=(dc == 0), stop=(dc == 7))
                    TT(P, LG[:, half * 8:(half + 1) * 8, :], b[:, 0:288].rearrange("p (a b) -> p a b", b=36),
                       brt[:].unsqueeze(1).to_broadcast([128, 8, 36]), ALU.add)
                Gl = LG[:, :, 0:4]
                gmax, gsum, pg, m1, m2, dsh, ex, w1, w2, g1, g2, d1f = [sm[:, k, :] for k in range(12)]
                RED(P, gmax, Gl, ALU.max)
                TT(P, G4[:], Gl, gmax.unsqueeze(2).to_broadcast([128, 16, 4]), ALU.subtract)
                TT(P, gm[:], Gl, gmax.unsqueeze(2).to_broadcast([128, 16, 4]), ALU.is_equal)
                ACT(P, G4[:], G4[:], AF.Exp)
                RED(P, gsum, G4[:], ALU.add)
                RCP(P, pg, gsum)
                TS(P, gm[:], gm[:], BIG, ALU.mult, -BIG, ALU.add)
                TT(P, EM[:].rearrange("p a (g e) -> p a g e", e=8), LG[:, :, 4:36].rearrange("p a (g e) -> p a g e", e=8),
                   gm[:].unsqueeze(3).to_broadcast([128, 16, 4, 8]), ALU.add)
                RED(P, m1, EM[:], ALU.max)
                TT(P, mk1[:], EM[:], m1.unsqueeze(2).to_broadcast([128, 16, 32]), ALU.is_equal)
                STT(P, EM2[:].rearrange("p a b -> p (a b)"), mk1[:].rearrange("p a b -> p (a b)"), -BIG,
                    EM[:].rearrange("p a b -> p (a b)"), ALU.mult, ALU.add)
                RED(P, m2, EM2[:], ALU.max)
                TT(P, mk2[:], EM2[:], m2.unsqueeze(2).to_broadcast([128, 16, 32]), ALU.is_equal)
                TT(P, dsh, m2, m1, ALU.subtract)
                ACT(P, ex, dsh, AF.Exp)
                TS(P, w1, ex, 1.0, ALU.add)
                RCP(P, w1, w1)
                TT(P, w2, ex, w1, ALU.mult)
                TT(P, g1, pg, w1, ALU.mult)
                TT(P, g2, pg, w2, ALU.mult)
                TT(P, selb[:], mk1[:], mk2[:], ALU.add)
                bp = nb()
                for i in range(NBLK):
                    for j in range(i):
                        MM(P, bp[:, i * 32:(i + 1) * 32], onesb[:], selb[:, j, :], start=(j == 0), stop=False)
                    MM(P, bp[:, i * 32:(i + 1) * 32], tri[:], selb[:, i, :], start=(i == 0), stop=True)
                TT(P, slot[:], bp[:].rearrange("p (a b) -> p a b", b=32),
                   ecoff[:].unsqueeze(1).to_broadcast([128, 16, 32]), ALU.add)
                TT(P, tmp3[:], slot[:], mk1[:], ALU.mult)
                RED(P, d1f, tmp3[:], ALU.add)
                CP(P, di[:, 0, :], d1f)
                TT(P, tmp3[:], slot[:], mk2[:], ALU.mult)
                RED(P, d1f, tmp3[:], ALU.add)
                CP(P, di[:, 1, :], d1f)
                XS_all = XS_s[:, :]
                regc = {}

                def bnd(e):
                    if "r" not in regc:
                        regc["r"] = e.to_reg(NSLOT - 1)
                    return regc["r"]

                for i in range(NBLK):
                    for k in range(2):
                        idx = di[:, k, i:i + 1]
                        src = x1b[:, i, :]
                        P.add("pool", (lambda idx, src: (lambda e: e.indirect_dma_start(
                            out=XS_all, out_offset=bass.IndirectOffsetOnAxis(ap=idx, axis=0), in_=src, in_offset=None,
                            bounds_check=bnd(e), oob_is_err=False)))(idx, src),
                            [idx, src], [XS_all], dma_key="scat")
                for e in range(NE):
                    s = e % 2
                    DMA(P, xg[s][:], XS_s[e * CAP:(e + 1) * CAP, :].rearrange("(a p) d -> p a d", p=128), f"xg{s}")
                    for a in range(2):
                        for hf in range(2):
                            b = nb()
                            bb = b[:, 0:256].bitcast(BF16)
                            for hh in range(4):
                                dc = hf * 4 + hh
                                TR(P, bb[:, hh * 128:(hh + 1) * 128], xg[s][:, a, dc * 128:(dc + 1) * 128], ident[:])
                            CP(P, xeT[s][:, hf * 4:(hf + 1) * 4, a * 128:(a + 1) * 128],
                               bb.rearrange("p (h e) -> p h e", e=128), eng=("dve" if hf else "act"))
                    for fc in range(4):
                        bg = nb()
                        for dc in range(8):
                            MM(P, bg[:, 0:CAP], EW[s][:, 0, dc, fc * 128:(fc + 1) * 128], xeT[s][:, dc, :],
                               start=(dc == 0), stop=(dc == 7))
                        bu = nb()
                        for dc in range(8):
                            MM(P, bu[:, 0:CAP], EW[s][:, 1, dc, fc * 128:(fc + 1) * 128], xeT[s][:, dc, :],
                               start=(dc == 0), stop=(dc == 7))
                        ACT(P, sg[fc % 2][:], bg[:, 0:CAP], AF.Silu)
                        TT(P, hT[s][:, fc, :], sg[fc % 2][:], bu[:, 0:CAP], ALU.mult)
                    wdv = EW[s][:, 2].rearrange("p (a b) n -> p a (b n)", b=2)
                    for a in range(2):
                        Y = yst[a]
                        for hf in range(2):
                            b = nb()
                            for fc in range(4):
                                MM(P, b[:], hT[s][:, fc, a * 128:(a + 1) * 128], wdv[:, fc, hf * 512:(hf + 1) * 512],
                                   start=(fc == 0), stop=(fc == 3))
                            CP(P, Y[:, hf * 512:(hf + 1) * 512], b[:], eng=("dve" if hf else "act"))
                        DMA(P, YS_s[e * CAP + a * 128:e * CAP + (a + 1) * 128, :], Y[:], Y.name)
                    if e + 2 < NE:
                        load_e(e + 2)
                YS_all = YS_s[:, :]
                for i in range(NBLK):
                    s = i % 2
                    for k, Yk in ((0, y1[s]), (1, y2[s])):
                        idx = di[:, k, i:i + 1]
                        dst = Yk[:, :]
                        P.add("pool", (lambda idx, dst: (lambda e: e.indirect_dma_start(
                            out=dst, out_offset=None, in_=YS_all, in_offset=bass.IndirectOffsetOnAxis(ap=idx, axis=0),
                            bounds_check=bnd(e), oob_is_err=False)))(idx, dst),
                            [idx, YS_all], [dst], dma_key=Yk.name)
                    X = xf[s]
                    DMA(P, X[:], X1F_s[i * 128:(i + 1) * 128, :], X.name)
                    TS(P, X[:], X[:], ALPHA, ALU.mult)
                    STT(P, X[:], y1[s][:], g1[:, i:i + 1], X[:], ALU.mult, ALU.add)
                    STT(P, X[:], y2[s][:], g2[:, i:i + 1], X[:], ALU.mult, ALU.add)
                    for hf in range(2):
                        P.add("dve", (lambda o, a: (lambda e: e.bn_stats(out=o, in_=a)))(bst[s][:, hf, :], X[:, hf * 512:(hf + 1) * 512]),
                              [X[:, hf * 512:(hf + 1) * 512]], [bst[s][:, hf, :]])
                    M = mv[s]
                    P.add("dve", (lambda o, a: (lambda e: e.bn_aggr(out=o, in_=a)))(M[:, 0:2], bst[s][:]),
                          [bst[s][:]], [M[:, 0:2]])
                    ACT(P, M[:, 2:3], M[:, 1:2], AF.Sqrt, bias=epsc[:, 0:1])
                    RCP(P, M[:, 3:4], M[:, 2:3])
                    TS(P, X[:], X[:], M[:, 0:1], ALU.subtract, M[:, 3:4], ALU.mult)
                    TT(P, X[:], X[:], lnp[:, 0, :], ALU.mult, eng="pool")
                    TT(P, X[:], X[:], lnp[:, 1, :], ALU.add)
                    DMA(P, y[i * 128:(i + 1) * 128, :], X[:], X.name)
                run_phase(g, P)
    return nc


def _bucket_table():
    dist = np.arange(0, 256, dtype=np.int32)
    max_exact = 16
    d = np.maximum(dist, 1).astype(np.float32)
    v = (np.log(d / np.float32(max_exact)) / np.float32(math.log(128 / max_exact)) * np.float32(32 - max_exact))
    large = max_exact + v.astype(np.float32).astype(np.int32)
    large = np.minimum(large, 31)
    return np.where(dist < max_exact, dist, large)


def _col(v, n):
    return np.ascontiguousarray(np.asarray(v, np.float32).reshape(n, 128).T)


def prepare_inputs(x, w_in, b_in, diff_lambda, head_norm_g, w_o_attn, rel_bias, conv_w, conv_b,
                   conv_ln_g, conv_ln_b, w_conv_out, b_conv_out, w_out, ln1_g, ln1_b,
                   router_g_w, router_g_b, router_e_w, router_e_b, expert_w_gate, expert_w_up,
                   expert_w_down, ln2_g, ln2_b):
    f = lambda a: np.asarray(a, np.float32)
    x = f(x)
    w_in0 = np.ascontiguousarray(f(w_in)[0])
    b_in0 = f(b_in)[0]
    rel = f(rel_bias)
    bucket = _bucket_table()
    kk = np.arange(128)[:, None]
    qq = np.arange(128)[None, :]
    d0 = qq - kk
    tile_diag = np.where(d0[None] >= 0, rel[bucket[np.maximum(d0, 0)]].transpose(2, 0, 1), np.float32(-1e30))
    tile_prev = rel[bucket[d0 + 128]].transpose(2, 0, 1)
    tile_far = np.broadcast_to(rel[31][:, None, None], (8, 128, 128))
    tile_mask = np.full((8, 128, 128), -1e30, np.float32)
    shared = {
        "w_in": w_in0,
        "bcol": _col(b_in0, 56),
        "bv_bc": np.ascontiguousarray(np.broadcast_to(b_in0[2048:3072], (128, 1024))),
        "lam_bc": np.ascontiguousarray(np.broadcast_to(f(diff_lambda)[0].reshape(256), (128, 256))),
        "hng_bc": np.ascontiguousarray(np.broadcast_to(f(head_norm_g)[0], (128, 128))),
        "relc": np.ascontiguousarray(np.broadcast_to(rel[31], (128, 8))),
        "ident": np.eye(128, dtype=np.float32),
        "tri": np.triu(np.ones((128, 128), np.float32), 1),
        "ecoff": np.ascontiguousarray(np.broadcast_to((np.arange(32) * CAP).astype(np.float32), (128, 32))),
        "convw": np.ascontiguousarray(f(conv_w)[0].reshape(31, 8, 128).transpose(2, 1, 0).reshape(128, 8 * 31)),
        "cvec": np.ascontiguousarray(np.concatenate([_col(f(conv_b)[0], 8), _col(f(conv_ln_g)[0], 8),
                                                     _col(f(conv_ln_b)[0], 8), _col(f(b_conv_out)[0], 8)], axis=1)),
        "w_o_attn": np.ascontiguousarray(f(w_o_attn)[0]),
        "w_conv_out": np.ascontiguousarray(f(w_conv_out)[0]),
        "w_out": np.ascontiguousarray(f(w_out)[0]),
        "lnp": np.ascontiguousarray(np.concatenate([np.broadcast_to(f(v)[0], (128, 1024))
                                                    for v in (ln1_g, ln1_b, ln2_g, ln2_b)], axis=1)),
        "w_r": np.ascontiguousarray(np.concatenate([f(router_g_w)[0], f(router_e_w)[0]], axis=1)),
        "b_r": np.ascontiguousarray(np.broadcast_to(np.concatenate([f(router_g_b)[0], f(router_e_b)[0]]), (128, 36))),
        "e_wg": np.ascontiguousarray(f(expert_w_gate)[0]),
        "e_wu": np.ascontiguousarray(f(expert_w_up)[0]),
        "e_wd": np.ascontiguousarray(f(expert_w_down)[0]),
    }
    in_maps = []
    for c in range(8):
        b, p = c // 2, c % 2
        blocks = x[b].reshape(32, 128, D)
        order = [2 * i + p for i in range(16)] + [2 * i + (1 - p) for i in range(16)]
        x_loc = blocks[order].reshape(S, D)
        halo = np.zeros((16, 32, D), np.float32)
        for i in range(16):
            start = (2 * i + p) * 128
            if start >= 32:
                halo[i] = x[b, start - 32:start]
        if p == 1:
            tiles = np.stack([tile_diag, tile_prev, tile_far], axis=1)
        else:
            tiles = np.stack([tile_diag, tile_mask, tile_prev], axis=1)
        m = dict(shared)
        m["xT"] = np.ascontiguousarray(x_loc.T)
        m["xhT"] = np.ascontiguousarray(halo.reshape(512, D).T)
        m["xtok"] = np.ascontiguousarray(x_loc[:TOWN])
        m["hm"] = np.full((128, 1), float(p), np.float32)
        m["biasT"] = np.ascontiguousarray(tiles.transpose(2, 0, 1, 3).reshape(128, 8 * 3 * 128).astype(np.float32))
        in_maps.append(m)
    return in_maps


def kernel(**inputs):
    in_maps = prepare_inputs(**inputs)
    nc = build_nc()
    res = run_bass_kernel_spmd(nc, in_maps, core_ids=list(range(8)))
    out = np.zeros((4, 32, 128, D), np.float32)
    for c in range(8):
        b, p = c // 2, c % 2
        yc = np.asarray(res.results[c]["y"]).reshape(16, 128, D)
        for i in range(16):
            out[b, 2 * i + p] = yc[i]
    return out.reshape(4, S, D)
```

```python
import contextlib
import math
import numpy as np
import concourse.bass as bass
import concourse.mybir as mybir
from concourse.bass_utils import run_bass_kernel_spmd

F32 = mybir.dt.float32
BF16 = mybir.dt.bfloat16
I32 = mybir.dt.int32
AF = mybir.ActivationFunctionType
ALU = mybir.AluOpType
AX = mybir.AxisListType

D = 1024
S = 4096
NBLK = 16
TOWN = 2048
H = 8
CAP = 256
NE = 32
NSLOT = NE * CAP
ALPHA = 2.0 ** 0.25
LN_EPS = 1e-5
LAM_INIT = 0.8 - 0.6 * math.exp(0.0)
BIG = 1.0e4

ENGINES = ("pe", "act", "dve", "pool", "sp")


def _region(ap):
    t = ap.tensor
    name = t.name
    dims = list(ap.ap)
    esz = mybir.dt.size(ap.dtype)
    if str(ap.space) == "DRAM":
        lo = ap.offset * esz
        hi = lo + (sum((c - 1) * abs(s) for s, c in dims) + 1) * esz
        return (name, 0, 1, lo, hi)
    pstep = dims[0][0]
    plo = ap.start_partition()
    phi = plo + ap.partition_size()
    off = ap.offset % pstep if pstep else ap.offset
    flo = off * esz
    fhi = flo + (sum((c - 1) * abs(s) for s, c in dims[1:]) + 1) * esz
    return (name, plo, phi, flo, fhi)


class Op:
    __slots__ = ("eng", "fn", "dma_key", "deps", "dma_need", "sig", "rank", "is_dma")

    def __init__(self, eng, fn, dma_key):
        self.eng = eng
        self.fn = fn
        self.dma_key = dma_key
        self.is_dma = dma_key is not None
        self.deps = []
        self.dma_need = {}
        self.sig = False
        self.rank = None


class Prog:
    def __init__(self, glob):
        self.g = glob
        self.ops = {e: [] for e in ENGINES}
        self.hist = {}

    def add(self, eng, fn, reads=(), writes=(), dma_key=None):
        rr = [a if isinstance(a, tuple) else _region(a) for a in reads]
        ww = [a if isinstance(a, tuple) else _region(a) for a in writes]
        op = Op(eng, fn, dma_key)
        self.ops[eng].append(op)
        g = self.g
        deps = {}
        for (name, plo, phi, flo, fhi) in rr:
            for rec in self.hist.setdefault(name, []):
                if rec[5] and rec[0] < phi and plo < rec[1] and rec[2] < fhi and flo < rec[3]:
                    deps[id(rec[4])] = rec[4]
        for (name, plo, phi, flo, fhi) in ww:
            keep = []
            for rec in self.hist.setdefault(name, []):
                if rec[0] < phi and plo < rec[1] and rec[2] < fhi and flo < rec[3]:
                    deps[id(rec[4])] = rec[4]
                    if plo <= rec[0] and rec[1] <= phi and flo <= rec[2] and rec[3] <= fhi:
                        continue
                keep.append(rec)
            self.hist[name] = keep
        for (name, plo, phi, flo, fhi) in rr:
            h = self.hist[name]
            if not op.is_dma:
                h[:] = [rec for rec in h if not ((not rec[5]) and rec[4].eng == eng and not rec[4].is_dma
                                                   and rec[0] == plo and rec[1] == phi
                                                   and rec[2] == flo and rec[3] == fhi)]
            h.append([plo, phi, flo, fhi, op, False])
        for (name, plo, phi, flo, fhi) in ww:
            self.hist[name].append([plo, phi, flo, fhi, op, True])
        deps.pop(id(op), None)
        for d in deps.values():
            if d.is_dma:
                op.dma_need[d.dma_key] = 16 * g.dma_count[d.dma_key]
        if dma_key is not None:
            g.dma_count[dma_key] = g.dma_count.get(dma_key, 0) + 1
        for d in deps.values():
            if d.is_dma:
                continue
            elif d.eng == "pe" and eng == "pe":
                continue
            else:
                op.deps.append(d)
                d.sig = True
        return op


class Glob:
    def __init__(self, nc, st):
        self.nc = nc
        self.dma_count = {}
        self.dma_sem = {}
        self.st = st
        self.esem = {e: st.enter_context(nc.semaphore(f"s_{e}")) for e in ENGINES}
        self.bar = st.enter_context(nc.semaphore("s_bar"))
        self.rank = {e: 0 for e in ENGINES}
        self.waited = {e: {} for e in ENGINES}
        self.nbar = 0

    def dsem(self, key):
        if key not in self.dma_sem:
            self.dma_sem[key] = self.st.enter_context(self.nc.semaphore(f"d_{len(self.dma_sem)}"))
        return self.dma_sem[key]


def run_phase(g, prog):
    nc = g.nc
    for e in ENGINES:
        for op in reversed(prog.ops[e]):
            if not op.is_dma:
                op.sig = True
                break
    for e in ENGINES:
        for op in prog.ops[e]:
            if op.sig and not op.is_dma:
                g.rank[e] += 1
                op.rank = g.rank[e]
    g.nbar += 1
    nbar = g.nbar
    for k in g.dma_count:
        g.dsem(k)

    def body(e, eng):
        waited = g.waited[e]
        last_rank = 0
        for op in prog.ops[e]:
            need = {}
            for d in op.deps:
                key = ("e", d.eng)
                if waited.get(key, 0) < d.rank and need.get(key, (None, 0))[1] < d.rank:
                    need[key] = (g.esem[d.eng], d.rank)
            for k, val in op.dma_need.items():
                key = ("d", k)
                if waited.get(key, 0) < val and need.get(key, (None, 0))[1] < val:
                    need[key] = (g.dsem(k), val)
            for key, (sem, val) in need.items():
                eng.wait_ge(sem, val)
                waited[key] = val
            ins = op.fn(eng)
            if op.is_dma:
                ins.then_inc(g.dsem(op.dma_key), 16)
            elif op.sig:
                ins.then_inc(g.esem[e], 1)
                last_rank = op.rank
        if last_rank and waited.get(("e", e), 0) < last_rank:
            eng.wait_ge(g.esem[e], last_rank)
            waited[("e", e)] = last_rank
        seen = set()
        for op in prog.ops[e]:
            if op.is_dma and op.dma_key not in seen:
                seen.add(op.dma_key)
                val = 16 * g.dma_count[op.dma_key]
                if waited.get(("d", op.dma_key), 0) < val:
                    eng.wait_ge(g.dsem(op.dma_key), val)
                    waited[("d", op.dma_key)] = val
        eng.sem_inc(g.bar, 1)
        eng.wait_ge(g.bar, 5 * nbar)

    with nc.Block() as block:
        @block.tensor
        def _(eng):
            body("pe", eng)

        @block.scalar
        def _(eng):
            body("act", eng)

        @block.vector
        def _(eng):
            body("dve", eng)

        @block.gpsimd
        def _(eng):
            body("pool", eng)

        @block.sync
        def _(eng):
            body("sp", eng)


def _isap(x):
    return not isinstance(x, (int, float)) and x is not None


def MM(P, out, lhsT, rhs, start=True, stop=True):
    P.add("pe", lambda e: e.matmul(out, lhsT, rhs, start=start, stop=stop), [lhsT, rhs], [out])


def TR(P, out, in_, ident):
    P.add("pe", lambda e: e.transpose(out, in_, ident), [in_, ident], [out])


def ACT(P, out, in_, func, bias=None, scale=1.0, accum_out=None):
    reads = [in_] + [a for a in (bias, scale) if _isap(a)]
    writes = [out] + ([accum_out] if accum_out is not None else [])
    kw = {}
    if bias is not None:
        kw["bias"] = bias
    if accum_out is not None:
        kw["accum_out"] = accum_out
    P.add("act", lambda e: e.activation(out=out, in_=in_, func=func, scale=scale, **kw), reads, writes)


def TT(P, out, in0, in1, op, eng="dve"):
    P.add(eng, lambda e: e.tensor_tensor(out=out, in0=in0, in1=in1, op=op), [in0, in1], [out])


def TS(P, out, in0, s1, op0, s2=None, op1=None, eng="dve"):
    reads = [in0] + [a for a in (s1, s2) if _isap(a)]
    if op1 is None:
        P.add(eng, lambda e: e.tensor_scalar(out=out, in0=in0, scalar1=s1, scalar2=None, op0=op0), reads, [out])
    else:
        P.add(eng, lambda e: e.tensor_scalar(out=out, in0=in0, scalar1=s1, scalar2=s2, op0=op0, op1=op1),
              reads, [out])


def STT(P, out, in0, scalar, in1, op0, op1):
    reads = [in0, in1] + ([scalar] if _isap(scalar) else [])
    P.add("dve", lambda e: e.scalar_tensor_tensor(out=out, in0=in0, scalar=scalar, in1=in1, op0=op0, op1=op1),
          reads, [out])


def CP(P, out, in_, eng="dve"):
    if eng == "act":
        P.add("act", lambda e: e.activation(out=out, in_=in_, func=AF.Copy), [in_], [out])
    else:
        P.add(eng, lambda e: e.tensor_copy(out=out, in_=in_), [in_], [out])


def RED(P, out, in_, op):
    P.add("dve", lambda e: e.tensor_reduce(out=out, in_=in_, op=op, axis=AX.X), [in_], [out])


def RCP(P, out, in_):
    P.add("dve", lambda e: e.reciprocal(out=out, in_=in_), [in_], [out])


def MSET(P, ap, val, eng="dve"):
    P.add(eng, lambda e: e.memset(ap, val), [], [ap])


def DMA(P, out, in_, key, eng="sp"):
    P.add(eng, lambda e: e.dma_start(out=out, in_=in_), [in_], [out], dma_key=key)


def build_nc(debug=False, stop_after=99):
    import os
    stop_after = int(os.environ.get('MK_STOP', stop_after))
    nc = bass.Bass("TRN2", target_bir_lowering=False)

    def inp(name, shape, dt=F32):
        return nc.dram_tensor(name, list(shape), dt, kind="ExternalInput").ap()

    def scr(name, shape, dt):
        return nc.dram_tensor(name, list(shape), dt, kind=("ExternalOutput" if debug else "Internal")).ap()

    xT = inp("xT", [D, S])
    xhT = inp("xhT", [D, 512])
    xtok = inp("xtok", [TOWN, D])
    w_in = inp("w_in", [D, 7168])
    bcol_d = inp("bcol", [128, 56])
    bv_d = inp("bv_bc", [128, 1024])
    hm_d = inp("hm", [128, 1])
    lam_d = inp("lam_bc", [128, 256])
    hng_d = inp("hng_bc", [128, 128])
    bias_d = inp("biasT", [128, 8 * 3 * 128])
    relc_d = inp("relc", [128, 8])
    ident_d = inp("ident", [128, 128])
    tri_d = inp("tri", [128, 128])
    ecoff_d = inp("ecoff", [128, 32])
    convw_d = inp("convw", [128, 8 * 31])
    cvec_d = inp("cvec", [128, 4 * 8])
    w_oa = inp("w_o_attn", [D, D])
    w_co = inp("w_conv_out", [D, D])
    w_out = inp("w_out", [D, D])
    lnp_d = inp("lnp", [128, 4 * 1024])
    wr_d = inp("w_r", [D, 36])
    br_d = inp("b_r", [128, 36])
    wg_d = inp("e_wg", [NE, D, 512])
    wu_d = inp("e_wu", [NE, D, 512])
    wd_d = inp("e_wd", [NE, 512, D])
    y = nc.dram_tensor("y", [TOWN, D], F32, kind="ExternalOutput").ap()

    KT_s = scr("KT_s", [H, 2, 128, S], BF16)
    QT_s = scr("QT_s", [H, 128, TOWN], BF16)
    VA_s = scr("VA_s", [32, 128, H * 256], BF16)
    G_s = scr("G_s", [16, 128, TOWN], BF16)
    H_s = scr("H_s", [8, 128, 16, 160], F32)
    X1F_s = scr("X1F_s", [TOWN, D], F32)
    XS_s = scr("XS_s", [NSLOT, D], BF16)
    YS_s = scr("YS_s", [NSLOT, D], F32)
    DBG_O = scr("DBG_O", [TOWN, D], F32) if debug else None
    DBG_C = scr("DBG_C", [D, TOWN], F32) if debug else None
    DBG_M = scr("DBG_M", [D, TOWN], F32) if debug else None

    with contextlib.ExitStack() as top:
        g = Glob(nc, top)
        ps = [top.enter_context(nc.psum_tensor(f"ps{i}", [128, 512], F32)) for i in range(8)]
        sb = lambda st, name, shape, dt: st.enter_context(nc.sbuf_tensor("s_" + name, list(shape), dt))
        ident = sb(top, "ident", [128, 128], BF16)
        cvec = sb(top, "cvec", [128, 32], F32)
        epsc = sb(top, "epsc", [128, 1], F32)

        if stop_after >= 1:
            with contextlib.ExitStack() as st:
                P = Prog(g)
                W = sb(st, "W", [128, 8, 7168], BF16)
                xt = [sb(st, f"xt{i}", [128, 8, 512], BF16) for i in range(2)]
                bcol = sb(st, "bcol", [128, 56], F32)
                bv = sb(st, "bv", [128, 1024], F32)
                hm = sb(st, "hm", [128, 1], F32)
                KA = [sb(st, f"KA{i}", [128, 512], BF16) for i in range(2)]
                KB = [sb(st, f"KB{i}", [128, 512], BF16) for i in range(2)]
                Qs = [sb(st, f"Qs{i}", [128, 512], BF16) for i in range(2)]
                Gs = [sb(st, f"Gs{i}", [128, 512], BF16) for i in range(3)]
                Hs = [sb(st, f"Hs{i}", [128, 512], F32) for i in range(2)]
                Sg = [sb(st, f"Sg{i}", [128, 512], F32) for i in range(2)]
                Vs = [sb(st, f"Vs{i}", [128, 8, 256], BF16) for i in range(2)]

                DMA(P, bcol[:], bcol_d, "bcol")
                DMA(P, bv[:], bv_d, "bv")
                DMA(P, hm[:], hm_d, "hm")
                DMA(P, cvec[:], cvec_d, "cvec")
                DMA(P, ident[:], ident_d, "ident", eng="pool")
                MSET(P, epsc[:], LN_EPS)
                for i in range(2):
                    MSET(P, KA[i][:], 0.0)
                    MSET(P, KB[i][:], 0.0)
                    MSET(P, Vs[i][:], 1.0)
                w_v = w_in.rearrange("(c p) n -> p c n", p=128)
                xT_v = xT.rearrange("(c p) t -> p c t", p=128)
                xhT_v = xhT.rearrange("(c p) t -> p c t", p=128)
                DMA(P, xt[0][:], xT_v[:, :, 0:512], "xt0", eng="pool")
                for grp in (1, 2, 0, 3, 4, 5, 6):
                    DMA(P, W[:, :, grp * 1024:(grp + 1) * 1024], w_v[:, :, grp * 1024:(grp + 1) * 1024],
                        f"W{grp}", eng="pool")
                bank = [0]

                def nb():
                    b = ps[bank[0] % 8]
                    bank[0] += 1
                    return b

                cnt = {"k": 0, "q": 0, "g": 0, "h": 0, "v": 0}
                import os
                PARTS = os.environ.get('PH1_PARTS', 'kvqgh')
                for t in range(9):
                    if t >= int(os.environ.get('PH1_T', '9')):
                        break
                    X = xt[t % 2]
                    if t + 1 < 9:
                        src = xT_v[:, :, (t + 1) * 512:(t + 2) * 512] if t + 1 < 8 else xhT_v
                        DMA(P, xt[(t + 1) % 2][:], src, f"xt{(t + 1) % 2}", eng="pool")

                    def proj(ch):
                        b = nb()
                        for dc in range(8):
                            MM(P, b[:], W[:, dc, ch * 128:(ch + 1) * 128], X[:, dc, :], start=(dc == 0), stop=(dc == 7))
                        return b

                    if t < 8 and 'k' in PARTS:
                        for hh in range(8):
                            b = proj(8 + hh)
                            ka, kb = KA[cnt["k"] % 2], KB[cnt["k"] % 2]
                            cnt["k"] += 1
                            TS(P, ka[0:64, :], b[0:64, :], bcol[0:64, 8 + hh:9 + hh], ALU.add)
                            ACT(P, kb[64:128, :], b[64:128, :], AF.Identity, bias=bcol[64:128, 8 + hh:9 + hh])
                            DMA(P, KT_s[hh, 0, :, t * 512:(t + 1) * 512], ka[:], ka.name)
                            DMA(P, KT_s[hh, 1, :, t * 512:(t + 1) * 512], kb[:], kb.name)
                        for blk in range(4 if 'v' in PARTS else 0):
                            vs = Vs[cnt["v"] % 2]
                            cnt["v"] += 1
                            for hf in range(2):
                                b = nb()
                                for dc in range(8):
                                    MM(P, b[:], X[:, dc, blk * 128:(blk + 1) * 128],
                                       W[:, dc, 2048 + hf * 512:2048 + (hf + 1) * 512], start=(dc == 0), stop=(dc == 7))
                                TT(P, vs[:, hf * 4:(hf + 1) * 4, 0:128], b[:].rearrange("p (h e) -> p h e", e=128),
                                   bv[:, hf * 512:(hf + 1) * 512].rearrange("p (h e) -> p h e", e=128), ALU.add)
                            if os.environ.get('VDMA', '1') == '1':
                                DMA(P, VA_s[t * 4 + blk], vs[:].rearrange("p h e -> p (h e)"), vs.name)
                    if t < 4 and 'q' in PARTS:
                        for hh in range(8):
                            b = proj(hh)
                            qs = Qs[cnt["q"] % 2]
                            cnt["q"] += 1
                            TS(P, qs[:], b[:], bcol[:, hh:hh + 1], ALU.add, 0.125, ALU.mult)
                            DMA(P, QT_s[hh, :, t * 512:(t + 1) * 512], qs[:], qs.name)
                        for c in range(16 if 'g' in PARTS else 0):
                            b = proj(40 + c)
                            gs = Gs[cnt["g"] % 3]
                            cnt["g"] += 1
                            ACT(P, gs[:], b[:], AF.Sigmoid, bias=bcol[:, 40 + c:41 + c])
                            DMA(P, G_s[c, :, t * 512:(t + 1) * 512], gs[:], gs.name)
                    if (t < 4 or t == 8) and 'h' in PARTS:
                        for c in range(8):
                            ba = proj(24 + c)
                            bg = proj(32 + c)
                            sg = Sg[cnt["h"] % 2]
                            hs = Hs[cnt["h"] % 2]
                            cnt["h"] += 1
                            ACT(P, sg[:], bg[:], AF.Sigmoid, bias=bcol[:, 32 + c:33 + c])
                            STT(P, hs[:], ba[:], bcol[:, 24 + c:25 + c], sg[:], ALU.add, ALU.mult)
                            if t == 8:
                                TS(P, hs[:, 0:32], hs[:, 0:32], hm[:, 0:1], ALU.mult)
                                DMA(P, H_s[c, :, :, 0:32], hs[:].rearrange("p (b e) -> p b e", e=32), hs.name)
                            else:
                                DMA(P, H_s[c, :, t * 4:(t + 1) * 4, 32:160],
                                    hs[:].rearrange("p (b e) -> p b e", e=128), hs.name)
                run_phase(g, P)

        conv_out = sb(top, "conv_out", [128, 8, TOWN], BF16)
        O_all = sb(top, "O_all", [128, NBLK, D], BF16)
        if stop_after >= 2:
            with contextlib.ExitStack() as st:
                P = Prog(g)
                KT = [sb(st, f"KT{i}", [128, 2, S], BF16) for i in range(2)]
                QT = [sb(st, f"QT{i}", [128, TOWN], BF16) for i in range(2)]
                VA = [sb(st, f"VA{i}", [128, 32, 256], BF16) for i in range(2)]
                PT = [sb(st, f"PT{i}", [128, 512], BF16) for i in range(3)]
                bias_f = sb(st, "bias_f", [128, 8, 384], F32)
                bias_b = sb(st, "bias_b", [128, 8, 3, 128], BF16)
                relc = sb(st, "relc", [128, 8], F32)
                lamb = sb(st, "lamb", [128, 4, 64], F32)
                lt = sb(st, "lt", [128, 8], F32)
                gv = sb(st, "gv", [128, 128], F32)
                hb = [sb(st, f"hb{i}", [128, 16, 160], F32) for i in range(2)]
                acc = sb(st, "acc", [128, 16, 128], F32)
                convw = sb(st, "convw", [128, 8, 31], F32)
                rr_ = [sb(st, f"rr{i}", [128, 8], F32) for i in range(2)]
                o1 = [sb(st, f"o1_{i}", [128, 128], F32) for i in range(2)]
                o2 = [sb(st, f"o2_{i}", [128, 128], F32) for i in range(2)]
                junk = sb(st, "junk", [128, 128], F32)

                DMA(P, bias_f[:], bias_d.rearrange("p (h x) -> p h x", x=384), "bias_f")
                DMA(P, relc[:], relc_d, "relc")
                DMA(P, lamb[:], lam_d.rearrange("p (a b) -> p a b", b=64), "lamb")
                DMA(P, gv[:], hng_d, "gv")
                DMA(P, convw[:], convw_d.rearrange("p (c k) -> p c k", k=31), "convw")
                for h in range(8):
                    TS(P, bias_b[:, h].rearrange("p a b -> p (a b)"), bias_f[:, h, :], relc[:, h:h + 1], ALU.subtract)
                TT(P, lamb[:, 0, :], lamb[:, 0, :], lamb[:, 1, :], ALU.mult)
                TT(P, lamb[:, 2, :], lamb[:, 2, :], lamb[:, 3, :], ALU.mult)
                RED(P, lt[:, 0:1], lamb[:, 0, :], ALU.add)
                RED(P, lt[:, 1:2], lamb[:, 2, :], ALU.add)
                ACT(P, lt[:, 2:4], lt[:, 0:2], AF.Exp)
                TT(P, lt[:, 4:5], lt[:, 3:4], lt[:, 2:3], ALU.subtract)
                TS(P, lt[:, 4:5], lt[:, 4:5], -LAM_INIT, ALU.add)
                TS(P, gv[:], gv[:], 1.0 - LAM_INIT, ALU.mult)

                def load_head(h):
                    s = h % 2
                    DMA(P, KT[s][:], KT_s[h].rearrange("m p t -> p m t"), f"KT{s}", eng="pool")
                    DMA(P, QT[s][:], QT_s[h], f"QT{s}", eng="pool")
                    DMA(P, VA[s][:], VA_s[:, :, h * 256:(h + 1) * 256].rearrange("b p e -> p b e"), f"VA{s}", eng="pool")

                def load_h(c):
                    DMA(P, hb[c % 2][:], H_s[c], f"hb{c % 2}", eng="pool")

                load_head(0)
                load_h(0)
                groups = []
                for h in range(8):
                    for i in range(NBLK):
                        lst = [(j, None) for j in range(i)] + [(16 + j, None) for j in range(i - 1)]
                        if i >= 1:
                            lst.append((16 + i - 1, 2))
                        lst.append((16 + i, 1))
                        lst.append((i, 0))
                        for m in range(2):
                            chunks = [lst[a:a + 4] for a in range(0, len(lst), 4)]
                            for ci, ch in enumerate(chunks):
                                groups.append((h, i, m, ch, ci == 0, ci == len(chunks) - 1))
                conv_ops = []

                def emit_qk(n):
                    h, i, m, ch, first, last = groups[n]
                    s = h % 2
                    Sb = ps[n % 3]
                    for j, (kb, bt) in enumerate(ch):
                        MM(P, Sb[:, j * 128:(j + 1) * 128], KT[s][:, m, kb * 128:(kb + 1) * 128],
                           QT[s][:, i * 128:(i + 1) * 128], start=True, stop=(bt is None))
                        if bt is not None:
                            MM(P, Sb[:, j * 128:(j + 1) * 128], ident[:], bias_b[:, h, bt, :], start=False, stop=True)
                    w = len(ch) * 128
                    ACT(P, PT[n % 3][:, 0:w], Sb[:, 0:w], AF.Exp)

                def emit_av(n):
                    h, i, m, ch, first, last = groups[n]
                    s = h % 2
                    par = (h * NBLK + i) % 2
                    Ob = ps[3 + 2 * par + m]
                    for j, (kb, bt) in enumerate(ch):
                        MM(P, Ob[:, 0:130], PT[n % 3][:, j * 128:(j + 1) * 128], VA[s][:, kb, 0:130],
                           start=(first and j == 0), stop=(last and j == len(ch) - 1))
                    if last and m == 1:
                        combine(h, i, par)

                def combine(h, i, par):
                    Oa, Ob = ps[3 + 2 * par], ps[4 + 2 * par]
                    r = rr_[par]
                    RCP(P, r[:, 0:1], Oa[:, 128:129])
                    RCP(P, r[:, 1:2], Ob[:, 128:129])
                    TS(P, r[:, 2:3], r[:, 1:2], lt[:, 4:5], ALU.mult)
                    TS(P, o1[par][:], Oa[:, 0:128], r[:, 0:1], ALU.mult)
                    STT(P, o2[par][:], Ob[:, 0:128], r[:, 2:3], o1[par][:], ALU.mult, ALU.add)
                    ACT(P, junk[:], o2[par][:], AF.Square, accum_out=r[:, 3:4])
                    ACT(P, r[:, 4:5], r[:, 3:4], AF.Ln, bias=epsc[:, 0:1], scale=1.0 / 128.0)
                    ACT(P, r[:, 5:6], r[:, 4:5], AF.Exp, scale=-0.5)
                    STT(P, O_all[:, i, h * 128:(h + 1) * 128], o2[par][:], r[:, 5:6], gv[:], ALU.mult, ALU.mult)
                    c = h
                    for k in (2 * i, 2 * i + 1):
                        if k > 30:
                            continue
                        src = hb[c % 2][:, :, 2 + k:2 + k + 128]
                        if k == 0:
                            TS(P, acc[:], src, convw[:, c, 0:1], ALU.mult, cvec[:, c:c + 1], ALU.add)
                        elif k < 30:
                            STT(P, acc[:], src, convw[:, c, k:k + 1], acc[:], ALU.mult, ALU.add)
                        else:
                            STT(P, conv_out[:, c, :].rearrange("p (b e) -> p b e", e=128), src,
                                convw[:, c, k:k + 1], acc[:], ALU.mult, ALU.add)

                prev_h = 0
                for n in range(len(groups) + 1):
                    newhead = n < len(groups) and groups[n][0] != prev_h
                    if n < len(groups):
                        emit_qk(n)
                    if n >= 1:
                        emit_av(n - 1)
                    if n == 0 or newhead:
                        prev_h = groups[n][0]
                        if prev_h + 1 < 8:
                            load_head(prev_h + 1)
                            load_h(prev_h + 1)
                if debug:
                    for i in range(NBLK):
                        DMA(P, DBG_O[i * 128:(i + 1) * 128, :], O_all[:, i, :], "dbg0", eng="pool")
                    for c in range(8):
                        DMA(P, DBG_C[c * 128:(c + 1) * 128, :], conv_out[:, c, :], "dbg1", eng="pool")
                run_phase(g, P)

        if stop_after >= 3:
            with contextlib.ExitStack() as st:
                P = Prog(g)
                oT = sb(st, "oT", [128, 8, TOWN], BF16)
                mT = sb(st, "mT", [128, 8, TOWN], BF16)
                ones = sb(st, "ones", [128, 128], BF16)
                Wc = [sb(st, f"Wc{i}", [128, 2, 8, 128], BF16) for i in range(2)]
                Wo = sb(st, "Wo", [128, 8, D], BF16)
                lnp = sb(st, "lnp", [128, 2, D], F32)
                sq = [sb(st, f"sq{i}", [128, 512], BF16) for i in range(2)]
                mean = sb(st, "mean", [128, 512], F32)
                rstd = sb(st, "rstd", [128, 512], F32)
                t1 = [sb(st, f"t1_{i}", [128, 512], F32) for i in range(2)]
                t2 = [sb(st, f"t2_{i}", [128, 512], F32) for i in range(2)]
                gab = [sb(st, f"gab{i}", [128, 2, 512], BF16) for i in range(2)]
                xb = [sb(st, f"xb{i}", [128, D], F32) for i in range(2)]
                z = [sb(st, f"z{i}", [128, D], F32) for i in range(2)]
                bst = [sb(st, f"bst{i}", [128, 2, 6], F32) for i in range(2)]
                mv = [sb(st, f"mv{i}", [128, 4], F32) for i in range(2)]
                bank = [0]

                def nb():
                    b = ps[bank[0] % 8]
                    bank[0] += 1
                    return b

                MSET(P, ones[:], 1.0 / 1024.0)
                DMA(P, lnp[:], lnp_d[:, 0:2048].rearrange("p (a b) -> p a b", b=1024), "lnp")
                DMA(P, Wo[:], w_out.rearrange("(c p) n -> p c n", p=128), "Wo", eng="pool")

                def load_wc(c):
                    s = c % 2
                    DMA(P, Wc[s][:, 0], w_oa.rearrange("(c p) n -> p c n", p=128)[:, :, c * 128:(c + 1) * 128],
                        f"Wc{s}", eng="pool")
                    DMA(P, Wc[s][:, 1], w_co.rearrange("(c p) n -> p c n", p=128)[:, :, c * 128:(c + 1) * 128],
                        f"Wc{s}", eng="pool")

                load_wc(0)
                for i in range(NBLK):
                    for hf in range(2):
                        b = nb()
                        bb = b[:, 0:256].bitcast(BF16)
                        for hh in range(4):
                            h = hf * 4 + hh
                            TR(P, bb[:, hh * 128:(hh + 1) * 128], O_all[:, i, h * 128:(h + 1) * 128], ident[:])
                        CP(P, oT[:, hf * 4:(hf + 1) * 4, i * 128:(i + 1) * 128],
                           bb.rearrange("p (h e) -> p h e", e=128), eng=("dve" if hf else "act"))
                for t in range(4):
                    sl = slice(t * 512, (t + 1) * 512)
                    bm = nb()
                    for c in range(8):
                        MM(P, bm[:], ones[:], conv_out[:, c, sl], start=(c == 0), stop=(c == 7))
                    be = nb()
                    for c in range(8):
                        s_ = sq[c % 2]
                        TT(P, s_[:], conv_out[:, c, sl], conv_out[:, c, sl], ALU.mult)
                        MM(P, be[:], ones[:], s_[:], start=(c == 0), stop=(c == 7))
                    CP(P, mean[:], bm[:])
                    TT(P, rstd[:], mean[:], mean[:], ALU.mult)
                    TT(P, rstd[:], be[:], rstd[:], ALU.subtract)
                    ACT(P, rstd[:], rstd[:], AF.Sqrt, bias=epsc[:, 0:1])
                    RCP(P, rstd[:], rstd[:])
                    for c in range(8):
                        a_ = t1[c % 2]
                        TT(P, a_[:], conv_out[:, c, sl], mean[:], ALU.subtract)
                        TT(P, a_[:], a_[:], rstd[:], ALU.mult)
                        ACT(P, conv_out[:, c, sl], a_[:], AF.Silu, bias=cvec[:, 16 + c:17 + c], scale=cvec[:, 8 + c:9 + c])
                wv = w_in.rearrange("(c p) n -> p c n", p=128)
                for c in range(8):
                    if c + 1 < 8:
                        load_wc(c + 1)
                    for t in range(4):
                        sl = slice(t * 512, (t + 1) * 512)
                        gb_ = gab[(c * 4 + t) % 2]
                        DMA(P, gb_[:, 0, :], G_s[c, :, sl], gb_.name, eng="pool")
                        DMA(P, gb_[:, 1, :], G_s[8 + c, :, sl], gb_.name, eng="pool")
                        ba = nb()
                        for vc in range(8):
                            MM(P, ba[:], Wc[c % 2][:, 0, vc, :], oT[:, vc, sl], start=(vc == 0), stop=(vc == 7))
                        bb = nb()
                        for vc in range(8):
                            MM(P, bb[:], Wc[c % 2][:, 1, vc, :], conv_out[:, vc, sl], start=(vc == 0), stop=(vc == 7))
                        a_ = t1[t % 2]
                        b_ = t2[t % 2]
                        TT(P, a_[:], ba[:], gb_[:, 0, :], ALU.mult)
                        STT(P, b_[:], bb[:], cvec[:, 24 + c:25 + c], gb_[:, 1, :], ALU.add, ALU.mult)
                        TT(P, mT[:, c, sl], a_[:], b_[:], ALU.add)
                if debug:
                    for c in range(8):
                        DMA(P, DBG_M[c * 128:(c + 1) * 128, :], mT[:, c, :], "dbg2", eng="pool")
                DMA(P, xb[0][:], xtok[0:128, :], xb[0].name)
                for i in range(NBLK):
                    X = xb[i % 2]
                    Z = z[i % 2]
                    if i + 1 < NBLK:
                        DMA(P, xb[(i + 1) % 2][:], xtok[(i + 1) * 128:(i + 2) * 128, :], xb[(i + 1) % 2].name)
                    for hf in range(2):
                        b = nb()
                        for dc in range(8):
                            MM(P, b[:], mT[:, dc, i * 128:(i + 1) * 128], Wo[:, dc, hf * 512:(hf + 1) * 512],
                               start=(dc == 0), stop=(dc == 7))
                        STT(P, Z[:, hf * 512:(hf + 1) * 512], X[:, hf * 512:(hf + 1) * 512], ALPHA, b[:], ALU.mult, ALU.add)
                        P.add("dve", (lambda o, a: (lambda e: e.bn_stats(out=o, in_=a)))(bst[i % 2][:, hf, :], Z[:, hf * 512:(hf + 1) * 512]),
                              [Z[:, hf * 512:(hf + 1) * 512]], [bst[i % 2][:, hf, :]])
                    M = mv[i % 2]
                    P.add("dve", (lambda o, a: (lambda e: e.bn_aggr(out=o, in_=a)))(M[:, 0:2], bst[i % 2][:]),
                          [bst[i % 2][:]], [M[:, 0:2]])
                    ACT(P, M[:, 2:3], M[:, 1:2], AF.Sqrt, bias=epsc[:, 0:1])
                    RCP(P, M[:, 3:4], M[:, 2:3])
                    TS(P, Z[:], Z[:], M[:, 0:1], ALU.subtract, M[:, 3:4], ALU.mult)
                    TT(P, Z[:], Z[:], lnp[:, 0, :], ALU.mult)
                    TT(P, Z[:], Z[:], lnp[:, 1, :], ALU.add)
                    CP(P, O_all[:, i, :], Z[:], eng="act")
                    DMA(P, X1F_s[i * 128:(i + 1) * 128, :], Z[:], Z.name)
                run_phase(g, P)

        if stop_after >= 4:
            x1b = O_all
            with contextlib.ExitStack() as st:
                P = Prog(g)
                x1T = conv_out
                wr = sb(st, "wr", [128, 8, 36], BF16)
                brt = sb(st, "brt", [128, 36], F32)
                tri = sb(st, "tri", [128, 128], BF16)
                onesb = sb(st, "onesb", [128, 128], BF16)
                ecoff = sb(st, "ecoff", [128, 32], F32)
                LG = sb(st, "LG", [128, 16, 36], F32)
                EM = sb(st, "EM", [128, 16, 32], F32)
                EM2 = sb(st, "EM2", [128, 16, 32], F32)
                mk1 = sb(st, "mk1", [128, 16, 32], F32)
                mk2 = sb(st, "mk2", [128, 16, 32], F32)
                slot = sb(st, "slot", [128, 16, 32], F32)
                tmp3 = sb(st, "tmp3", [128, 16, 32], F32)
                selb = sb(st, "selb", [128, 16, 32], BF16)
                G4 = sb(st, "G4", [128, 16, 4], F32)
                gm = sb(st, "gm", [128, 16, 4], F32)
                sm = sb(st, "sm", [128, 12, 16], F32)
                di = sb(st, "di", [128, 2, 16], I32)
                EW = [sb(st, f"EW{i}", [128, 3, 8, 512], BF16) for i in range(2)]
                xg = [sb(st, f"xg{i}", [128, 2, D], BF16) for i in range(2)]
                xeT = [sb(st, f"xeT{i}", [128, 8, CAP], BF16) for i in range(2)]
                hT = [sb(st, f"hT{i}", [128, 4, CAP], BF16) for i in range(2)]
                sg = [sb(st, f"sgm{i}", [128, CAP], F32) for i in range(2)]
                yst = [sb(st, f"yst{i}", [128, D], F32) for i in range(2)]
                lnp = sb(st, "lnp2", [128, 2, D], F32)
                y1 = [sb(st, f"y1_{i}", [128, D], F32) for i in range(2)]
                y2 = [sb(st, f"y2_{i}", [128, D], F32) for i in range(2)]
                xf = [sb(st, f"xf{i}", [128, D], F32) for i in range(2)]
                bst = [sb(st, f"bs2{i}", [128, 2, 6], F32) for i in range(2)]
                mv = [sb(st, f"mv2{i}", [128, 4], F32) for i in range(2)]
                bank = [0]

                def nb():
                    b = ps[bank[0] % 8]
                    bank[0] += 1
                    return b

                def load_e(e):
                    s = e % 2
                    DMA(P, EW[s][:, 0], wg_d[e].rearrange("(c p) n -> p c n", p=128), f"EWa{s}", eng="pool")
                    DMA(P, EW[s][:, 1], wu_d[e].rearrange("(c p) n -> p c n", p=128), f"EWb{s}", eng="pool")
                    DMA(P, EW[s][:, 2].rearrange("p (a b) n -> p a (b n)", b=2),
                        wd_d[e].rearrange("(c p) n -> p c n", p=128), f"EWc{s}", eng="pool")

                DMA(P, wr[:], wr_d.rearrange("(c p) n -> p c n", p=128), "wr", eng="pool")
                DMA(P, tri[:], tri_d, "tri", eng="pool")
                DMA(P, brt[:], br_d, "brt")
                DMA(P, ecoff[:], ecoff_d, "ecoff")
                DMA(P, lnp[:], lnp_d[:, 2048:4096].rearrange("p (a b) -> p a b", b=1024), "lnp2")
                MSET(P, onesb[:], 1.0)
                zt = sb(st, "zt", [128, 4096], BF16)
                MSET(P, zt[:], 0.0, eng="pool")
                for zi in range(16):
                    DMA(P, XS_s[zi * 512:(zi + 1) * 512, :].rearrange("(p a) d -> p (a d)", p=128), zt[:], "zt")
                load_e(0)
                load_e(1)
                for i in range(NBLK):
                    for hf in range(2):
                        b = nb()
                        bb = b[:, 0:256].bitcast(BF16)
                        for hh in range(4):
                            dc = hf * 4 + hh
                            TR(P, bb[:, hh * 128:(hh + 1) * 128], x1b[:, i, dc * 128:(dc + 1) * 128], ident[:])
                        CP(P, x1T[:, hf * 4:(hf + 1) * 4, i * 128:(i + 1) * 128],
                           bb.rearrange("p (h e) -> p h e", e=128), eng=("dve" if hf else "act"))
                for half in range(2):
                    b = nb()
                    for ii in range(8):
                        i = half * 8 + ii
                        for dc in range(8):
                            MM(P, b[:, ii * 36:(ii + 1) * 36], x1T[:, dc, i * 128:(i + 1) * 128], wr[:, dc, :],
                               start=(dc == 0), stop=(dc == 7))
                    TT(P, LG[:, half * 8:(half + 1) * 8, :], b[:, 0:288].rearrange("p (a b) -> p a b", b=36),
                       brt[:].unsqueeze(1).to_broadcast([128, 8, 36]), ALU.add)
                Gl = LG[:, :, 0:4]
                gmax, gsum, pg, m1, m2, dsh, ex, w1, w2, g1, g2, d1f = [sm[:, k, :] for k in range(12)]
                RED(P, gmax, Gl, ALU.max)
                TT(P, G4[:], Gl, gmax.unsqueeze(2).to_broadcast([128, 16, 4]), ALU.subtract)
                TT(P, gm[:], Gl, gmax.unsqueeze(2).to_broadcast([128, 16, 4]), ALU.is_equal)
                ACT(P, G4[:], G4[:], AF.Exp)
                RED(P, gsum, G4[:], ALU.add)
                RCP(P, pg, gsum)
                TS(P, gm[:], gm[:], BIG, ALU.mult, -BIG, ALU.add)
                TT(P, EM[:].rearrange("p a (g e) -> p a g e", e=8), LG[:, :, 4:36].rearrange("p a (g e) -> p a g e", e=8),
                   gm[:].unsqueeze(3).to_broadcast([128, 16, 4, 8]), ALU.add)
                RED(P, m1, EM[:], ALU.max)
                TT(P, mk1[:], EM[:], m1.unsqueeze(2).to_broadcast([128, 16, 32]), ALU.is_equal)
                STT(P, EM2[:].rearrange("p a b -> p (a b)"), mk1[:].rearrange("p a b -> p (a b)"), -BIG,
                    EM[:].rearrange("p a b -> p (a b)"), ALU.mult, ALU.add)
                RED(P, m2, EM2[:], ALU.max)
                TT(P, mk2[:], EM2[:], m2.unsqueeze(2).to_broadcast([128, 16, 32]), ALU.is_equal)
                TT(P, dsh, m2, m1, ALU.subtract)
                ACT(P, ex, dsh, AF.Exp)
                TS(P, w1, ex, 1.0, ALU.add)
                RCP(P, w1, w1)
                TT(P, w2, ex, w1, ALU.mult)
                TT(P, g1, pg, w1, ALU.mult)
                TT(P, g2, pg, w2, ALU.mult)
                TT(P, selb[:], mk1[:], mk2[:], ALU.add)
                bp = nb()
                for i in range(NBLK):
                    for j in range(i):
                        MM(P, bp[:, i * 32:(i + 1) * 32], onesb[:], selb[:, j, :], start=(j == 0), stop=False)
                    MM(P, bp[:, i * 32:(i + 1) * 32], tri[:], selb[:, i, :], start=(i == 0), stop=True)
                TT(P, slot[:], bp[:].rearrange("p (a b) -> p a b", b=32),
                   ecoff[:].unsqueeze(1).to_broadcast([128, 16, 32]), ALU.add)
                TT(P, tmp3[:], slot[:], mk1[:], ALU.mult)
                RED(P, d1f, tmp3[:], ALU.add)
                CP(P, di[:, 0, :], d1f)
                TT(P, tmp3[:], slot[:], mk2[:], ALU.mult)
                RED(P, d1f, tmp3[:], ALU.add)
                CP(P, di[:, 1, :], d1f)
                XS_all = XS_s[:, :]
                regc = {}

                def bnd(e):
                    if "r" not in regc:
                        regc["r"] = e.to_reg(NSLOT - 1)
                    return regc["r"]

                for i in range(NBLK):
                    for k in range(2):
                        idx = di[:, k, i:i + 1]
                        src = x1b[:, i, :]
                        P.add("pool", (lambda idx, src: (lambda e: e.indirect_dma_start(
                            out=XS_all, out_offset=bass.IndirectOffsetOnAxis(ap=idx, axis=0), in_=src, in_offset=None,
                            bounds_check=bnd(e), oob_is_err=False)))(idx, src),
                            [idx, src], [XS_all], dma_key="scat")
                DMA(P, xg[0][:], XS_s[0:CAP, :].rearrange("(a p) d -> p a d", p=128), "xg0")
                for e in range(NE):
                    s = e % 2
                    if e + 1 < NE:
                        DMA(P, xg[(e + 1) % 2][:], XS_s[(e + 1) * CAP:(e + 2) * CAP, :].rearrange("(a p) d -> p a d", p=128),
                            f"xg{(e + 1) % 2}")
                    for a in range(2):
                        for hf in range(2):
                            b = nb()
                            bb = b[:, 0:256].bitcast(BF16)
                            for hh in range(4):
                                dc = hf * 4 + hh
                                TR(P, bb[:, hh * 128:(hh + 1) * 128], xg[s][:, a, dc * 128:(dc + 1) * 128], ident[:])
                            CP(P, xeT[s][:, hf * 4:(hf + 1) * 4, a * 128:(a + 1) * 128],
                               bb.rearrange("p (h e) -> p h e", e=128), eng=("dve" if hf else "act"))
                    for fc in range(4):
                        bg = nb()
                        for dc in range(8):
                            MM(P, bg[:, 0:CAP], EW[s][:, 0, dc, fc * 128:(fc + 1) * 128], xeT[s][:, dc, :],
                               start=(dc == 0), stop=(dc == 7))
                        bu = nb()
                        for dc in range(8):
                            MM(P, bu[:, 0:CAP], EW[s][:, 1, dc, fc * 128:(fc + 1) * 128], xeT[s][:, dc, :],
                               start=(dc == 0), stop=(dc == 7))
                        ACT(P, sg[fc % 2][:], bg[:, 0:CAP], AF.Silu)
                        TT(P, hT[s][:, fc, :], sg[fc % 2][:], bu[:, 0:CAP], ALU.mult)
                    wdv = EW[s][:, 2].rearrange("p (a b) n -> p a (b n)", b=2)
                    for a in range(2):
                        Y = yst[a]
                        for hf in range(2):
                            b = nb()
                            for fc in range(4):
                                MM(P, b[:], hT[s][:, fc, a * 128:(a + 1) * 128], wdv[:, fc, hf * 512:(hf + 1) * 512],
                                   start=(fc == 0), stop=(fc == 3))
                            CP(P, Y[:, hf * 512:(hf + 1) * 512], b[:], eng=("dve" if hf else "act"))
                        DMA(P, YS_s[e * CAP + a * 128:e * CAP + (a + 1) * 128, :], Y[:], Y.name)
                    if e + 2 < NE:
                        load_e(e + 2)
                YS_all = YS_s[:, :]
                def fetch(i):
                    s = i % 2
                    for k, Yk in ((0, y1[s]), (1, y2[s])):
                        idx = di[:, k, i:i + 1]
                        dst = Yk[:, :]
                        P.add("pool", (lambda idx, dst: (lambda e: e.indirect_dma_start(
                            out=dst, out_offset=None, in_=YS_all, in_offset=bass.IndirectOffsetOnAxis(ap=idx, axis=0),
                            bounds_check=bnd(e), oob_is_err=False)))(idx, dst),
                            [idx, YS_all], [dst], dma_key=Yk.name)
                    DMA(P, xf[s][:], X1F_s[i * 128:(i + 1) * 128, :], xf[s].name)

                fetch(0)
                for i in range(NBLK):
                    s = i % 2
                    X = xf[s]
                    if i + 1 < NBLK:
                        fetch(i + 1)
                    TS(P, X[:], X[:], ALPHA, ALU.mult)
                    STT(P, X[:], y1[s][:], g1[:, i:i + 1], X[:], ALU.mult, ALU.add)
                    STT(P, X[:], y2[s][:], g2[:, i:i + 1], X[:], ALU.mult, ALU.add)
                    for hf in range(2):
                        P.add("dve", (lambda o, a: (lambda e: e.bn_stats(out=o, in_=a)))(bst[s][:, hf, :], X[:, hf * 512:(hf + 1) * 512]),
                              [X[:, hf * 512:(hf + 1) * 512]], [bst[s][:, hf, :]])
                    M = mv[s]
                    P.add("dve", (lambda o, a: (lambda e: e.bn_aggr(out=o, in_=a)))(M[:, 0:2], bst[s][:]),
                          [bst[s][:]], [M[:, 0:2]])
                    ACT(P, M[:, 2:3], M[:, 1:2], AF.Sqrt, bias=epsc[:, 0:1])
                    RCP(P, M[:, 3:4], M[:, 2:3])
                    TS(P, X[:], X[:], M[:, 0:1], ALU.subtract, M[:, 3:4], ALU.mult)
                    TT(P, X[:], X[:], lnp[:, 0, :], ALU.mult)
                    TT(P, X[:], X[:], lnp[:, 1, :], ALU.add)
                    DMA(P, y[i * 128:(i + 1) * 128, :], X[:], X.name)
                run_phase(g, P)
    return nc


def _bucket_table():
    dist = np.arange(0, 256, dtype=np.int32)
    max_exact = 16
    d = np.maximum(dist, 1).astype(np.float32)
    v = (np.log(d / np.float32(max_exact)) / np.float32(math.log(128 / max_exact)) * np.float32(32 - max_exact))
    large = max_exact + v.astype(np.float32).astype(np.int32)
    large = np.minimum(large, 31)
    return np.where(dist < max_exact, dist, large)


def _col(v, n):
    return np.ascontiguousarray(np.asarray(v, np.float32).reshape(n, 128).T)


def prepare_inputs(x, w_in, b_in, diff_lambda, head_norm_g, w_o_attn, rel_bias, conv_w, conv_b,
                   conv_ln_g, conv_ln_b, w_conv_out, b_conv_out, w_out, ln1_g, ln1_b,
                   router_g_w, router_g_b, router_e_w, router_e_b, expert_w_gate, expert_w_up,
                   expert_w_down, ln2_g, ln2_b):
    f = lambda a: np.asarray(a, np.float32)
    x = f(x)
    w_in0 = np.ascontiguousarray(f(w_in)[0])
    b_in0 = f(b_in)[0]
    rel = f(rel_bias)
    bucket = _bucket_table()
    kk = np.arange(128)[:, None]
    qq = np.arange(128)[None, :]
    d0 = qq - kk
    tile_diag = np.where(d0[None] >= 0, rel[bucket[np.maximum(d0, 0)]].transpose(2, 0, 1), np.float32(-1e30))
    tile_prev = rel[bucket[d0 + 128]].transpose(2, 0, 1)
    tile_far = np.broadcast_to(rel[31][:, None, None], (8, 128, 128))
    tile_mask = np.full((8, 128, 128), -1e30, np.float32)
    shared = {
        "w_in": w_in0,
        "bcol": _col(b_in0, 56),
        "bv_bc": np.ascontiguousarray(np.broadcast_to(b_in0[2048:3072], (128, 1024))),
        "lam_bc": np.ascontiguousarray(np.broadcast_to(f(diff_lambda)[0].reshape(256), (128, 256))),
        "hng_bc": np.ascontiguousarray(np.broadcast_to(f(head_norm_g)[0], (128, 128))),
        "relc": np.ascontiguousarray(np.broadcast_to(rel[31], (128, 8))),
        "ident": np.eye(128, dtype=np.float32),
        "tri": np.triu(np.ones((128, 128), np.float32), 1),
        "ecoff": np.ascontiguousarray(np.broadcast_to((np.arange(32) * CAP).astype(np.float32), (128, 32))),
        "convw": np.ascontiguousarray(f(conv_w)[0].reshape(31, 8, 128).transpose(2, 1, 0).reshape(128, 8 * 31)),
        "cvec": np.ascontiguousarray(np.concatenate([_col(f(conv_b)[0], 8), _col(f(conv_ln_g)[0], 8),
                                                     _col(f(conv_ln_b)[0], 8), _col(f(b_conv_out)[0], 8)], axis=1)),
        "w_o_attn": np.ascontiguousarray(f(w_o_attn)[0]),
        "w_conv_out": np.ascontiguousarray(f(w_conv_out)[0]),
        "w_out": np.ascontiguousarray(f(w_out)[0]),
        "lnp": np.ascontiguousarray(np.concatenate([np.broadcast_to(f(v)[0], (128, 1024))
                                                    for v in (ln1_g, ln1_b, ln2_g, ln2_b)], axis=1)),
        "w_r": np.ascontiguousarray(np.concatenate([f(router_g_w)[0], f(router_e_w)[0]], axis=1)),
        "b_r": np.ascontiguousarray(np.broadcast_to(np.concatenate([f(router_g_b)[0], f(router_e_b)[0]]), (128, 36))),
        "e_wg": np.ascontiguousarray(f(expert_w_gate)[0]),
        "e_wu": np.ascontiguousarray(f(expert_w_up)[0]),
        "e_wd": np.ascontiguousarray(f(expert_w_down)[0]),
    }
    in_maps = []
    for c in range(8):
        b, p = c // 2, c % 2
        blocks = x[b].reshape(32, 128, D)
        order = [2 * i + p for i in range(16)] + [2 * i + (1 - p) for i in range(16)]
        x_loc = blocks[order].reshape(S, D)
        halo = np.zeros((16, 32, D), np.float32)
        for i in range(16):
            start = (2 * i + p) * 128
            if start >= 32:
                halo[i] = x[b, start - 32:start]
        if p == 1:
            tiles = np.stack([tile_diag, tile_prev, tile_far], axis=1)
        else:
            tiles = np.stack([tile_diag, tile_mask, tile_prev], axis=1)
        m = dict(shared)
        m["xT"] = np.ascontiguousarray(x_loc.T)
        m["xhT"] = np.ascontiguousarray(halo.reshape(512, D).T)
        m["xtok"] = np.ascontiguousarray(x_loc[:TOWN])
        m["hm"] = np.full((128, 1), float(p), np.float32)
        m["biasT"] = np.ascontiguousarray(tiles.transpose(2, 0, 1, 3).reshape(128, 8 * 3 * 128).astype(np.float32))
        in_maps.append(m)
    return in_maps


def kernel(**inputs):
    in_maps = prepare_inputs(**inputs)
    nc = build_nc()
    res = run_bass_kernel_spmd(nc, in_maps, core_ids=list(range(8)))
    out = np.zeros((4, 32, 128, D), np.float32)
    for c in range(8):
        b, p = c // 2, c % 2
        yc = np.asarray(res.results[c]["y"]).reshape(16, 128, D)
        for i in range(16):
            out[b, 2 * i + p] = yc[i]
    return out.reshape(4, S, D)
```

```python
import contextlib
import math
import numpy as np
import concourse.bass as bass
import concourse.mybir as mybir
from concourse.bass_utils import run_bass_kernel_spmd

F32 = mybir.dt.float32
BF16 = mybir.dt.bfloat16
I32 = mybir.dt.int32
AF = mybir.ActivationFunctionType
ALU = mybir.AluOpType
AX = mybir.AxisListType

D = 1024
S = 4096
NBLK = 16
TOWN = 2048
H = 8
CAP = 256
NE = 32
NSLOT = NE * CAP
ALPHA = 2.0 ** 0.25
LN_EPS = 1e-5
LAM_INIT = 0.8 - 0.6 * math.exp(0.0)
BIG = 1.0e4

ENGINES = ("pe", "act", "dve", "pool", "sp")


def _region(ap):
    t = ap.tensor
    name = t.name
    dims = list(ap.ap)
    esz = mybir.dt.size(ap.dtype)
    if str(ap.space) == "DRAM":
        lo = ap.offset * esz
        hi = lo + (sum((c - 1) * abs(s) for s, c in dims) + 1) * esz
        return (name, 0, 1, lo, hi)
    pstep = dims[0][0]
    plo = ap.start_partition()
    phi = plo + ap.partition_size()
    off = ap.offset % pstep if pstep else ap.offset
    flo = off * esz
    fhi = flo + (sum((c - 1) * abs(s) for s, c in dims[1:]) + 1) * esz
    return (name, plo, phi, flo, fhi)


class Op:
    __slots__ = ("eng", "fn", "dma_key", "deps", "dma_need", "sig", "rank", "is_dma")

    def __init__(self, eng, fn, dma_key):
        self.eng = eng
        self.fn = fn
        self.dma_key = dma_key
        self.is_dma = dma_key is not None
        self.deps = []
        self.dma_need = {}
        self.sig = False
        self.rank = None


class Prog:
    def __init__(self, glob):
        self.g = glob
        self.ops = {e: [] for e in ENGINES}
        self.hist = {}

    def add(self, eng, fn, reads=(), writes=(), dma_key=None):
        rr = [a if isinstance(a, tuple) else _region(a) for a in reads]
        ww = [a if isinstance(a, tuple) else _region(a) for a in writes]
        op = Op(eng, fn, dma_key)
        self.ops[eng].append(op)
        g = self.g
        deps = {}
        for (name, plo, phi, flo, fhi) in rr:
            for rec in self.hist.setdefault(name, []):
                if rec[5] and rec[0] < phi and plo < rec[1] and rec[2] < fhi and flo < rec[3]:
                    deps[id(rec[4])] = rec[4]
        for (name, plo, phi, flo, fhi) in ww:
            keep = []
            for rec in self.hist.setdefault(name, []):
                if rec[0] < phi and plo < rec[1] and rec[2] < fhi and flo < rec[3]:
                    deps[id(rec[4])] = rec[4]
                    if plo <= rec[0] and rec[1] <= phi and flo <= rec[2] and rec[3] <= fhi:
                        continue
                keep.append(rec)
            self.hist[name] = keep
        for (name, plo, phi, flo, fhi) in rr:
            h = self.hist[name]
            if not op.is_dma:
                h[:] = [rec for rec in h if not ((not rec[5]) and rec[4].eng == eng and not rec[4].is_dma
                                                   and rec[0] == plo and rec[1] == phi
                                                   and rec[2] == flo and rec[3] == fhi)]
            h.append([plo, phi, flo, fhi, op, False])
        for (name, plo, phi, flo, fhi) in ww:
            self.hist[name].append([plo, phi, flo, fhi, op, True])
        deps.pop(id(op), None)
        for d in deps.values():
            if d.is_dma:
                op.dma_need[d.dma_key] = 16 * g.dma_count[d.dma_key]
        if dma_key is not None:
            g.dma_count[dma_key] = g.dma_count.get(dma_key, 0) + 1
        for d in deps.values():
            if d.is_dma:
                continue
            elif d.eng == "pe" and eng == "pe":
                continue
            else:
                op.deps.append(d)
                d.sig = True
        return op


class Glob:
    def __init__(self, nc, st):
        self.nc = nc
        self.dma_count = {}
        self.dma_sem = {}
        self.st = st
        self.esem = {e: st.enter_context(nc.semaphore(f"s_{e}")) for e in ENGINES}
        self.bar = st.enter_context(nc.semaphore("s_bar"))
        self.rank = {e: 0 for e in ENGINES}
        self.waited = {e: {} for e in ENGINES}
        self.nbar = 0

    def dsem(self, key):
        if key not in self.dma_sem:
            self.dma_sem[key] = self.st.enter_context(self.nc.semaphore(f"d_{len(self.dma_sem)}"))
        return self.dma_sem[key]


def run_phase(g, prog):
    nc = g.nc
    for e in ENGINES:
        for op in reversed(prog.ops[e]):
            if not op.is_dma:
                op.sig = True
                break
    for e in ENGINES:
        for op in prog.ops[e]:
            if op.sig and not op.is_dma:
                g.rank[e] += 1
                op.rank = g.rank[e]
    g.nbar += 1
    nbar = g.nbar
    for k in g.dma_count:
        g.dsem(k)

    def body(e, eng):
        waited = g.waited[e]
        last_rank = 0
        for op in prog.ops[e]:
            need = {}
            for d in op.deps:
                key = ("e", d.eng)
                if waited.get(key, 0) < d.rank and need.get(key, (None, 0))[1] < d.rank:
                    need[key] = (g.esem[d.eng], d.rank)
            for k, val in op.dma_need.items():
                key = ("d", k)
                if waited.get(key, 0) < val and need.get(key, (None, 0))[1] < val:
                    need[key] = (g.dsem(k), val)
            for key, (sem, val) in need.items():
                eng.wait_ge(sem, val)
                waited[key] = val
            ins = op.fn(eng)
            if op.is_dma:
                ins.then_inc(g.dsem(op.dma_key), 16)
            elif op.sig:
                ins.then_inc(g.esem[e], 1)
                last_rank = op.rank
        if last_rank and waited.get(("e", e), 0) < last_rank:
            eng.wait_ge(g.esem[e], last_rank)
            waited[("e", e)] = last_rank
        seen = set()
        for op in prog.ops[e]:
            if op.is_dma and op.dma_key not in seen:
                seen.add(op.dma_key)
                val = 16 * g.dma_count[op.dma_key]
                if waited.get(("d", op.dma_key), 0) < val:
                    eng.wait_ge(g.dsem(op.dma_key), val)
                    waited[("d", op.dma_key)] = val
        eng.sem_inc(g.bar, 1)
        eng.wait_ge(g.bar, 5 * nbar)

    with nc.Block() as block:
        @block.tensor
        def _(eng):
            body("pe", eng)

        @block.scalar
        def _(eng):
            body("act", eng)

        @block.vector
        def _(eng):
            body("dve", eng)

        @block.gpsimd
        def _(eng):
            body("pool", eng)

        @block.sync
        def _(eng):
            body("sp", eng)


def _isap(x):
    return not isinstance(x, (int, float)) and x is not None


def MM(P, out, lhsT, rhs, start=True, stop=True):
    P.add("pe", lambda e: e.matmul(out, lhsT, rhs, start=start, stop=stop), [lhsT, rhs], [out])


def TR(P, out, in_, ident):
    P.add("pe", lambda e: e.transpose(out, in_, ident), [in_, ident], [out])


def ACT(P, out, in_, func, bias=None, scale=1.0, accum_out=None):
    reads = [in_] + [a for a in (bias, scale) if _isap(a)]
    writes = [out] + ([accum_out] if accum_out is not None else [])
    kw = {}
    if bias is not None:
        kw["bias"] = bias
    if accum_out is not None:
        kw["accum_out"] = accum_out
    P.add("act", lambda e: e.activation(out=out, in_=in_, func=func, scale=scale, **kw), reads, writes)


def TT(P, out, in0, in1, op, eng="dve"):
    P.add(eng, lambda e: e.tensor_tensor(out=out, in0=in0, in1=in1, op=op), [in0, in1], [out])


def TS(P, out, in0, s1, op0, s2=None, op1=None, eng="dve"):
    reads = [in0] + [a for a in (s1, s2) if _isap(a)]
    if op1 is None:
        P.add(eng, lambda e: e.tensor_scalar(out=out, in0=in0, scalar1=s1, scalar2=None, op0=op0), reads, [out])
    else:
        P.add(eng, lambda e: e.tensor_scalar(out=out, in0=in0, scalar1=s1, scalar2=s2, op0=op0, op1=op1),
              reads, [out])


def STT(P, out, in0, scalar, in1, op0, op1):
    reads = [in0, in1] + ([scalar] if _isap(scalar) else [])
    P.add("dve", lambda e: e.scalar_tensor_tensor(out=out, in0=in0, scalar=scalar, in1=in1, op0=op0, op1=op1),
          reads, [out])


def CP(P, out, in_, eng="dve"):
    if eng == "act":
        P.add("act", lambda e: e.activation(out=out, in_=in_, func=AF.Copy), [in_], [out])
    else:
        P.add(eng, lambda e: e.tensor_copy(out=out, in_=in_), [in_], [out])


def RED(P, out, in_, op):
    P.add("dve", lambda e: e.tensor_reduce(out=out, in_=in_, op=op, axis=AX.X), [in_], [out])


def RCP(P, out, in_):
    P.add("dve", lambda e: e.reciprocal(out=out, in_=in_), [in_], [out])


def MSET(P, ap, val, eng="dve"):
    P.add(eng, lambda e: e.memset(ap, val), [], [ap])


def DMA(P, out, in_, key, eng="sp"):
    P.add(eng, lambda e: e.dma_start(out=out, in_=in_), [in_], [out], dma_key=key)


def build_nc(debug=False, stop_after=99):
    import os
    stop_after = int(os.environ.get('MK_STOP', stop_after))
    nc = bass.Bass("TRN2", target_bir_lowering=False)

    def inp(name, shape, dt=F32):
        return nc.dram_tensor(name, list(shape), dt, kind="ExternalInput").ap()

    def scr(name, shape, dt):
        return nc.dram_tensor(name, list(shape), dt, kind=("ExternalOutput" if debug else "Internal")).ap()

    xT = inp("xT", [D, S])
    xhT = inp("xhT", [D, 512])
    xtok = inp("xtok", [TOWN, D])
    w_in = inp("w_in", [D, 7168])
    bcol_d = inp("bcol", [128, 56])
    bv_d = inp("bv_bc", [128, 1024])
    hm_d = inp("hm", [128, 1])
    lam_d = inp("lam_bc", [128, 256])
    hng_d = inp("hng_bc", [128, 128])
    bias_d = inp("biasT", [128, 8 * 3 * 128])
    relc_d = inp("relc", [128, 8])
    ident_d = inp("ident", [128, 128])
    tri_d = inp("tri", [128, 128])
    ecoff_d = inp("ecoff", [128, 32])
    convw_d = inp("convw", [128, 8 * 31])
    cvec_d = inp("cvec", [128, 4 * 8])
    w_oa = inp("w_o_attn", [D, D])
    w_co = inp("w_conv_out", [D, D])
    w_out = inp("w_out", [D, D])
    lnp_d = inp("lnp", [128, 4 * 1024])
    wr_d = inp("w_r", [D, 36])
    br_d = inp("b_r", [128, 36])
    wg_d = inp("e_wg", [NE, D, 512])
    wu_d = inp("e_wu", [NE, D, 512])
    wd_d = inp("e_wd", [NE, 512, D])
    y = nc.dram_tensor("y", [TOWN, D], F32, kind="ExternalOutput").ap()

    KT_s = scr("KT_s", [H, 2, 128, S], BF16)
    QT_s = scr("QT_s", [H, 128, TOWN], BF16)
    VA_s = scr("VA_s", [32, 128, H * 256], BF16)
    G_s = scr("G_s", [16, 128, TOWN], BF16)
    H_s = scr("H_s", [8, 128, 16, 160], F32)
    X1F_s = scr("X1F_s", [TOWN, D], F32)
    XS_s = scr("XS_s", [NSLOT, D], BF16)
    YS_s = scr("YS_s", [NSLOT, D], F32)
    DBG_O = scr("DBG_O", [TOWN, D], F32) if debug else None
    DBG_C = scr("DBG_C", [D, TOWN], F32) if debug else None
    DBG_M = scr("DBG_M", [D, TOWN], F32) if debug else None

    with contextlib.ExitStack() as top:
        g = Glob(nc, top)
        ps = [top.enter_context(nc.psum_tensor(f"ps{i}", [128, 512], F32)) for i in range(8)]
        sb = lambda st, name, shape, dt: st.enter_context(nc.sbuf_tensor("s_" + name, list(shape), dt))
        ident = sb(top, "ident", [128, 128], BF16)
        cvec = sb(top, "cvec", [128, 32], F32)
        epsc = sb(top, "epsc", [128, 1], F32)

        if stop_after >= 1:
            with contextlib.ExitStack() as st:
                P = Prog(g)
                W = sb(st, "W", [128, 8, 7168], BF16)
                xt = [sb(st, f"xt{i}", [128, 8, 512], BF16) for i in range(2)]
                bcol = sb(st, "bcol", [128, 56], F32)
                bv = sb(st, "bv", [128, 1024], F32)
                hm = sb(st, "hm", [128, 1], F32)
                KA = [sb(st, f"KA{i}", [128, 512], BF16) for i in range(2)]
                KB = [sb(st, f"KB{i}", [128, 512], BF16) for i in range(2)]
                Qs = [sb(st, f"Qs{i}", [128, 512], BF16) for i in range(2)]
                Gs = [sb(st, f"Gs{i}", [128, 512], BF16) for i in range(3)]
                Hs = [sb(st, f"Hs{i}", [128, 512], F32) for i in range(2)]
                Sg = [sb(st, f"Sg{i}", [128, 512], F32) for i in range(2)]
                Vs = [sb(st, f"Vs{i}", [128, 8, 256], BF16) for i in range(2)]

                DMA(P, bcol[:], bcol_d, "bcol")
                DMA(P, bv[:], bv_d, "bv")
                DMA(P, hm[:], hm_d, "hm")
                DMA(P, cvec[:], cvec_d, "cvec")
                DMA(P, ident[:], ident_d, "ident", eng="pool")
                MSET(P, epsc[:], LN_EPS)
                for i in range(2):
                    MSET(P, KA[i][:], 0.0)
                    MSET(P, KB[i][:], 0.0)
                    MSET(P, Vs[i][:], 1.0)
                w_v = w_in.rearrange("(c p) n -> p c n", p=128)
                xT_v = xT.rearrange("(c p) t -> p c t", p=128)
                xhT_v = xhT.rearrange("(c p) t -> p c t", p=128)
                DMA(P, xt[0][:], xT_v[:, :, 0:512], "xt0", eng="pool")
                for grp in (1, 2, 0, 3, 4, 5, 6):
                    DMA(P, W[:, :, grp * 1024:(grp + 1) * 1024], w_v[:, :, grp * 1024:(grp + 1) * 1024],
                        f"W{grp}", eng="pool")
                bank = [0]

                def nb():
                    b = ps[bank[0] % 8]
                    bank[0] += 1
                    return b

                cnt = {"k": 0, "q": 0, "g": 0, "h": 0, "v": 0}
                import os
                PARTS = os.environ.get('PH1_PARTS', 'kvqgh')
                for t in range(9):
                    if t >= int(os.environ.get('PH1_T', '9')):
                        break
                    X = xt[t % 2]
                    if t + 1 < 9:
                        src = xT_v[:, :, (t + 1) * 512:(t + 2) * 512] if t + 1 < 8 else xhT_v
                        DMA(P, xt[(t + 1) % 2][:], src, f"xt{(t + 1) % 2}", eng="pool")

                    def proj(ch):
                        b = nb()
                        for dc in range(8):
                            MM(P, b[:], W[:, dc, ch * 128:(ch + 1) * 128], X[:, dc, :], start=(dc == 0), stop=(dc == 7))
                        return b

                    if t < 8 and 'k' in PARTS:
                        for hh in range(8):
                            b = proj(8 + hh)
                            ka, kb = KA[cnt["k"] % 2], KB[cnt["k"] % 2]
                            cnt["k"] += 1
                            TS(P, ka[0:64, :], b[0:64, :], bcol[0:64, 8 + hh:9 + hh], ALU.add)
                            ACT(P, kb[64:128, :], b[64:128, :], AF.Identity, bias=bcol[64:128, 8 + hh:9 + hh])
                            DMA(P, KT_s[hh, 0, :, t * 512:(t + 1) * 512], ka[:], ka.name)
                            DMA(P, KT_s[hh, 1, :, t * 512:(t + 1) * 512], kb[:], kb.name)
                        for blk in range(4 if 'v' in PARTS else 0):
                            vs = Vs[cnt["v"] % 2]
                            cnt["v"] += 1
                            for hf in range(2):
                                b = nb()
                                for dc in range(8):
                                    MM(P, b[:], X[:, dc, blk * 128:(blk + 1) * 128],
                                       W[:, dc, 2048 + hf * 512:2048 + (hf + 1) * 512], start=(dc == 0), stop=(dc == 7))
                                TT(P, vs[:, hf * 4:(hf + 1) * 4, 0:128], b[:].rearrange("p (h e) -> p h e", e=128),
                                   bv[:, hf * 512:(hf + 1) * 512].rearrange("p (h e) -> p h e", e=128), ALU.add)
                            if os.environ.get('VDMA', '1') == '1':
                                DMA(P, VA_s[t * 4 + blk], vs[:].rearrange("p h e -> p (h e)"), vs.name)
                    if t < 4 and 'q' in PARTS:
                        for hh in range(8):
                            b = proj(hh)
                            qs = Qs[cnt["q"] % 2]
                            cnt["q"] += 1
                            TS(P, qs[:], b[:], bcol[:, hh:hh + 1], ALU.add, 0.125, ALU.mult)
                            DMA(P, QT_s[hh, :, t * 512:(t + 1) * 512], qs[:], qs.name)
                        for c in range(16 if 'g' in PARTS else 0):
                            b = proj(40 + c)
                            gs = Gs[cnt["g"] % 3]
                            cnt["g"] += 1
                            ACT(P, gs[:], b[:], AF.Sigmoid, bias=bcol[:, 40 + c:41 + c])
                            DMA(P, G_s[c, :, t * 512:(t + 1) * 512], gs[:], gs.name)
                    if (t < 4 or t == 8) and 'h' in PARTS:
                        for c in range(8):
                            ba = proj(24 + c)
                            bg = proj(32 + c)
                            sg = Sg[cnt["h"] % 2]
                            hs = Hs[cnt["h"] % 2]
                            cnt["h"] += 1
                            ACT(P, sg[:], bg[:], AF.Sigmoid, bias=bcol[:, 32 + c:33 + c])
                            STT(P, hs[:], ba[:], bcol[:, 24 + c:25 + c], sg[:], ALU.add, ALU.mult)
                            if t == 8:
                                TS(P, hs[:, 0:32], hs[:, 0:32], hm[:, 0:1], ALU.mult)
                                DMA(P, H_s[c, :, :, 0:32], hs[:].rearrange("p (b e) -> p b e", e=32), hs.name)
                            else:
                                DMA(P, H_s[c, :, t * 4:(t + 1) * 4, 32:160],
                                    hs[:].rearrange("p (b e) -> p b e", e=128), hs.name)
                run_phase(g, P)

        conv_out = sb(top, "conv_out", [128, 8, TOWN], BF16)
        O_all = sb(top, "O_all", [128, NBLK, D], BF16)
        if stop_after >= 2:
            with contextlib.ExitStack() as st:
                P = Prog(g)
                KT = [sb(st, f"KT{i}", [128, 2, S], BF16) for i in range(2)]
                QT = [sb(st, f"QT{i}", [128, TOWN], BF16) for i in range(2)]
                VA = [sb(st, f"VA{i}", [128, 32, 256], BF16) for i in range(2)]
                PT = [sb(st, f"PT{i}", [128, 512], BF16) for i in range(3)]
                bias_f = sb(st, "bias_f", [128, 8, 384], F32)
                bias_b = sb(st, "bias_b", [128, 8, 3, 128], BF16)
                relc = sb(st, "relc", [128, 8], F32)
                lamb = sb(st, "lamb", [128, 4, 64], F32)
                lt = sb(st, "lt", [128, 8], F32)
                gv = sb(st, "gv", [128, 128], F32)
                hb = [sb(st, f"hb{i}", [128, 16, 160], F32) for i in range(2)]
                acc = sb(st, "acc", [128, 16, 128], F32)
                convw = sb(st, "convw", [128, 8, 31], F32)
                rr_ = [sb(st, f"rr{i}", [128, 8], F32) for i in range(2)]
                o1 = [sb(st, f"o1_{i}", [128, 128], F32) for i in range(2)]
                o2 = [sb(st, f"o2_{i}", [128, 128], F32) for i in range(2)]
                junk = sb(st, "junk", [128, 128], F32)

                DMA(P, bias_f[:], bias_d.rearrange("p (h x) -> p h x", x=384), "bias_f")
                DMA(P, relc[:], relc_d, "relc")
                DMA(P, lamb[:], lam_d.rearrange("p (a b) -> p a b", b=64), "lamb")
                DMA(P, gv[:], hng_d, "gv")
                DMA(P, convw[:], convw_d.rearrange("p (c k) -> p c k", k=31), "convw")
                for h in range(8):
                    TS(P, bias_b[:, h].rearrange("p a b -> p (a b)"), bias_f[:, h, :], relc[:, h:h + 1], ALU.subtract)
                TT(P, lamb[:, 0, :], lamb[:, 0, :], lamb[:, 1, :], ALU.mult)
                TT(P, lamb[:, 2, :], lamb[:, 2, :], lamb[:, 3, :], ALU.mult)
                RED(P, lt[:, 0:1], lamb[:, 0, :], ALU.add)
                RED(P, lt[:, 1:2], lamb[:, 2, :], ALU.add)
                ACT(P, lt[:, 2:4], lt[:, 0:2], AF.Exp)
                TT(P, lt[:, 4:5], lt[:, 3:4], lt[:, 2:3], ALU.subtract)
                TS(P, lt[:, 4:5], lt[:, 4:5], -LAM_INIT, ALU.add)
                TS(P, gv[:], gv[:], 1.0 - LAM_INIT, ALU.mult)

                def load_head(h):
                    s = h % 2
                    DMA(P, KT[s][:], KT_s[h].rearrange("m p t -> p m t"), f"KT{s}", eng="pool")
                    DMA(P, QT[s][:], QT_s[h], f"QT{s}", eng="pool")
                    DMA(P, VA[s][:], VA_s[:, :, h * 256:(h + 1) * 256].rearrange("b p e -> p b e"), f"VA{s}", eng="pool")

                def load_h(c):
                    DMA(P, hb[c % 2][:], H_s[c], f"hb{c % 2}", eng="pool")

                load_head(0)
                load_h(0)
                groups = []
                for h in range(8):
                    for i in range(NBLK):
                        lst = [(j, None) for j in range(i)] + [(16 + j, None) for j in range(i - 1)]
                        if i >= 1:
                            lst.append((16 + i - 1, 2))
                        lst.append((16 + i, 1))
                        lst.append((i, 0))
                        for m in range(2):
                            chunks = [lst[a:a + 4] for a in range(0, len(lst), 4)]
                            for ci, ch in enumerate(chunks):
                                groups.append((h, i, m, ch, ci == 0, ci == len(chunks) - 1))
                conv_ops = []

                def emit_qk(n):
                    h, i, m, ch, first, last = groups[n]
                    s = h % 2
                    Sb = ps[n % 3]
                    for j, (kb, bt) in enumerate(ch):
                        MM(P, Sb[:, j * 128:(j + 1) * 128], KT[s][:, m, kb * 128:(kb + 1) * 128],
                           QT[s][:, i * 128:(i + 1) * 128], start=True, stop=(bt is None))
                        if bt is not None:
                            MM(P, Sb[:, j * 128:(j + 1) * 128], ident[:], bias_b[:, h, bt, :], start=False, stop=True)
                    w = len(ch) * 128
                    ACT(P, PT[n % 3][:, 0:w], Sb[:, 0:w], AF.Exp)

                def emit_av(n):
                    h, i, m, ch, first, last = groups[n]
                    s = h % 2
                    par = (h * NBLK + i) % 2
                    Ob = ps[3 + 2 * par + m]
                    for j, (kb, bt) in enumerate(ch):
                        MM(P, Ob[:, 0:130], PT[n % 3][:, j * 128:(j + 1) * 128], VA[s][:, kb, 0:130],
                           start=(first and j == 0), stop=(last and j == len(ch) - 1))
                    if last and m == 1:
                        combine(h, i, par)

                def combine(h, i, par):
                    Oa, Ob = ps[3 + 2 * par], ps[4 + 2 * par]
                    r = rr_[par]
                    RCP(P, r[:, 0:1], Oa[:, 128:129])
                    RCP(P, r[:, 1:2], Ob[:, 128:129])
                    TS(P, r[:, 2:3], r[:, 1:2], lt[:, 4:5], ALU.mult)
                    TS(P, o1[par][:], Oa[:, 0:128], r[:, 0:1], ALU.mult)
                    STT(P, o2[par][:], Ob[:, 0:128], r[:, 2:3], o1[par][:], ALU.mult, ALU.add)
                    ACT(P, junk[:], o2[par][:], AF.Square, accum_out=r[:, 3:4])
                    ACT(P, r[:, 4:5], r[:, 3:4], AF.Ln, bias=epsc[:, 0:1], scale=1.0 / 128.0)
                    ACT(P, r[:, 5:6], r[:, 4:5], AF.Exp, scale=-0.5)
                    STT(P, O_all[:, i, h * 128:(h + 1) * 128], o2[par][:], r[:, 5:6], gv[:], ALU.mult, ALU.mult)
                    c = h
                    for k in (2 * i, 2 * i + 1):
                        if k > 30:
                            continue
                        src = hb[c % 2][:, :, 2 + k:2 + k + 128]
                        if k == 0:
                            TS(P, acc[:], src, convw[:, c, 0:1], ALU.mult, cvec[:, c:c + 1], ALU.add)
                        elif k < 30:
                            STT(P, acc[:], src, convw[:, c, k:k + 1], acc[:], ALU.mult, ALU.add)
                        else:
                            STT(P, conv_out[:, c, :].rearrange("p (b e) -> p b e", e=128), src,
                                convw[:, c, k:k + 1], acc[:], ALU.mult, ALU.add)

                prev_h = 0
                for n in range(len(groups) + 1):
                    newhead = n < len(groups) and groups[n][0] != prev_h
                    if n < len(groups):
                        emit_qk(n)
                    if n >= 1:
                        emit_av(n - 1)
                    if n == 0 or newhead:
                        prev_h = groups[n][0]
                        if prev_h + 1 < 8:
                            load_head(prev_h + 1)
                            load_h(prev_h + 1)
                if debug:
                    for i in range(NBLK):
                        DMA(P, DBG_O[i * 128:(i + 1) * 128, :], O_all[:, i, :], "dbg0", eng="pool")
                    for c in range(8):
                        DMA(P, DBG_C[c * 128:(c + 1) * 128, :], conv_out[:, c, :], "dbg1", eng="pool")
                run_phase(g, P)

        if stop_after >= 3:
            with contextlib.ExitStack() as st:
                P = Prog(g)
                oT = sb(st, "oT", [128, 8, TOWN], BF16)
                mT = sb(st, "mT", [128, 8, TOWN], BF16)
                ones = sb(st, "ones", [128, 128], BF16)
                Wc = [sb(st, f"Wc{i}", [128, 2, 8, 128], BF16) for i in range(2)]
                Wo = sb(st, "Wo", [128, 8, D], BF16)
                lnp = sb(st, "lnp", [128, 2, D], F32)
                sq = [sb(st, f"sq{i}", [128, 512], BF16) for i in range(2)]
                mean = sb(st, "mean", [128, 512], F32)
                rstd = sb(st, "rstd", [128, 512], F32)
                t1 = [sb(st, f"t1_{i}", [128, 512], F32) for i in range(2)]
                t2 = [sb(st, f"t2_{i}", [128, 512], F32) for i in range(2)]
                gab = [sb(st, f"gab{i}", [128, 2, 512], BF16) for i in range(2)]
                xb = [sb(st, f"xb{i}", [128, D], F32) for i in range(2)]
                z = [sb(st, f"z{i}", [128, D], F32) for i in range(2)]
                bst = [sb(st, f"bst{i}", [128, 2, 6], F32) for i in range(2)]
                mv = [sb(st, f"mv{i}", [128, 4], F32) for i in range(2)]
                bank = [0]

                def nb():
                    b = ps[bank[0] % 8]
                    bank[0] += 1
                    return b

                MSET(P, ones[:], 1.0 / 1024.0)
                DMA(P, lnp[:], lnp_d[:, 0:2048].rearrange("p (a b) -> p a b", b=1024), "lnp")
                DMA(P, Wo[:], w_out.rearrange("(c p) n -> p c n", p=128), "Wo", eng="pool")

                def load_wc(c):
                    s = c % 2
                    DMA(P, Wc[s][:, 0], w_oa.rearrange("(c p) n -> p c n", p=128)[:, :, c * 128:(c + 1) * 128],
                        f"Wc{s}", eng="pool")
                    DMA(P, Wc[s][:, 1], w_co.rearrange("(c p) n -> p c n", p=128)[:, :, c * 128:(c + 1) * 128],
                        f"Wc{s}", eng="pool")

                load_wc(0)

                def load_gab(n):
                    c_, t_ = n // 4, n % 4
                    gq = gab[n % 2]
                    DMA(P, gq[:, 0, :], G_s[c_, :, t_ * 512:(t_ + 1) * 512], gq.name, eng="pool")
                    DMA(P, gq[:, 1, :], G_s[8 + c_, :, t_ * 512:(t_ + 1) * 512], gq.name, eng="pool")

                for i in range(NBLK):
                    for hf in range(2):
                        b = nb()
                        bb = b[:, 0:256].bitcast(BF16)
                        for hh in range(4):
                            h = hf * 4 + hh
                            TR(P, bb[:, hh * 128:(hh + 1) * 128], O_all[:, i, h * 128:(h + 1) * 128], ident[:])
                        CP(P, oT[:, hf * 4:(hf + 1) * 4, i * 128:(i + 1) * 128],
                           bb.rearrange("p (h e) -> p h e", e=128), eng=("dve" if hf else "act"))
                for t in range(4):
                    sl = slice(t * 512, (t + 1) * 512)
                    bm = nb()
                    for c in range(8):
                        MM(P, bm[:], ones[:], conv_out[:, c, sl], start=(c == 0), stop=(c == 7))
                    be = nb()
                    for c in range(8):
                        s_ = sq[c % 2]
                        TT(P, s_[:], conv_out[:, c, sl], conv_out[:, c, sl], ALU.mult)
                        MM(P, be[:], ones[:], s_[:], start=(c == 0), stop=(c == 7))
                    CP(P, mean[:], bm[:])
                    TT(P, rstd[:], mean[:], mean[:], ALU.mult)
                    TT(P, rstd[:], be[:], rstd[:], ALU.subtract)
                    ACT(P, rstd[:], rstd[:], AF.Sqrt, bias=epsc[:, 0:1])
                    RCP(P, rstd[:], rstd[:])
                    for c in range(8):
                        a_ = t1[c % 2]
                        TT(P, a_[:], conv_out[:, c, sl], mean[:], ALU.subtract)
                        TT(P, a_[:], a_[:], rstd[:], ALU.mult)
                        ACT(P, conv_out[:, c, sl], a_[:], AF.Silu, bias=cvec[:, 16 + c:17 + c], scale=cvec[:, 8 + c:9 + c])
                wv = w_in.rearrange("(c p) n -> p c n", p=128)
                for c in range(8):
                    if c + 1 < 8:
                        load_wc(c + 1)
                    for t in range(4):
                        sl = slice(t * 512, (t + 1) * 512)
                        gb_ = gab[(c * 4 + t) % 2]
                        if c == 0 and t == 0:
                            load_gab(0)
                        if c * 4 + t + 1 < 32:
                            load_gab(c * 4 + t + 1)
                        ba = nb()
                        for vc in range(8):
                            MM(P, ba[:], Wc[c % 2][:, 0, vc, :], oT[:, vc, sl], start=(vc == 0), stop=(vc == 7))
                        bb = nb()
                        for vc in range(8):
                            MM(P, bb[:], Wc[c % 2][:, 1, vc, :], conv_out[:, vc, sl], start=(vc == 0), stop=(vc == 7))
                        a_ = t1[t % 2]
                        b_ = t2[t % 2]
                        TT(P, a_[:], ba[:], gb_[:, 0, :], ALU.mult)
                        STT(P, b_[:], bb[:], cvec[:, 24 + c:25 + c], gb_[:, 1, :], ALU.add, ALU.mult)
                        TT(P, mT[:, c, sl], a_[:], b_[:], ALU.add)
                if debug:
                    for c in range(8):
                        DMA(P, DBG_M[c * 128:(c + 1) * 128, :], mT[:, c, :], "dbg2", eng="pool")
                DMA(P, xb[0][:], xtok[0:128, :], xb[0].name)
                for i in range(NBLK):
                    X = xb[i % 2]
                    Z = z[i % 2]
                    if i + 1 < NBLK:
                        DMA(P, xb[(i + 1) % 2][:], xtok[(i + 1) * 128:(i + 2) * 128, :], xb[(i + 1) % 2].name)
                    for hf in range(2):
                        b = nb()
                        for dc in range(8):
                            MM(P, b[:], mT[:, dc, i * 128:(i + 1) * 128], Wo[:, dc, hf * 512:(hf + 1) * 512],
                               start=(dc == 0), stop=(dc == 7))
                        STT(P, Z[:, hf * 512:(hf + 1) * 512], X[:, hf * 512:(hf + 1) * 512], ALPHA, b[:], ALU.mult, ALU.add)
                        P.add("dve", (lambda o, a: (lambda e: e.bn_stats(out=o, in_=a)))(bst[i % 2][:, hf, :], Z[:, hf * 512:(hf + 1) * 512]),
                              [Z[:, hf * 512:(hf + 1) * 512]], [bst[i % 2][:, hf, :]])
                    M = mv[i % 2]
                    P.add("dve", (lambda o, a: (lambda e: e.bn_aggr(out=o, in_=a)))(M[:, 0:2], bst[i % 2][:]),
                          [bst[i % 2][:]], [M[:, 0:2]])
                    ACT(P, M[:, 2:3], M[:, 1:2], AF.Sqrt, bias=epsc[:, 0:1])
                    RCP(P, M[:, 3:4], M[:, 2:3])
                    TS(P, Z[:], Z[:], M[:, 0:1], ALU.subtract, M[:, 3:4], ALU.mult)
                    TT(P, Z[:], Z[:], lnp[:, 0, :], ALU.mult)
                    TT(P, Z[:], Z[:], lnp[:, 1, :], ALU.add)
                    CP(P, O_all[:, i, :], Z[:], eng="act")
                    DMA(P, X1F_s[i * 128:(i + 1) * 128, :], Z[:], Z.name)
                run_phase(g, P)

        if stop_after >= 4:
            x1b = O_all
            with contextlib.ExitStack() as st:
                P = Prog(g)
                x1T = conv_out
                wr = sb(st, "wr", [128, 8, 36], BF16)
                brt = sb(st, "brt", [128, 36], F32)
                tri = sb(st, "tri", [128, 128], BF16)
                onesb = sb(st, "onesb", [128, 128], BF16)
                ecoff = sb(st, "ecoff", [128, 32], F32)
                LG = sb(st, "LG", [128, 16, 36], F32)
                EM = sb(st, "EM", [128, 16, 32], F32)
                EM2 = sb(st, "EM2", [128, 16, 32], F32)
                mk1 = sb(st, "mk1", [128, 16, 32], F32)
                mk2 = sb(st, "mk2", [128, 16, 32], F32)
                slot = sb(st, "slot", [128, 16, 32], F32)
                tmp3 = sb(st, "tmp3", [128, 16, 32], F32)
                selb = sb(st, "selb", [128, 16, 32], BF16)
                G4 = sb(st, "G4", [128, 16, 4], F32)
                gm = sb(st, "gm", [128, 16, 4], F32)
                sm = sb(st, "sm", [128, 12, 16], F32)
                di = sb(st, "di", [128, 2, 16], I32)
                EW = [sb(st, f"EW{i}", [128, 3, 8, 512], BF16) for i in range(2)]
                xg = [sb(st, f"xg{i}", [128, 2, D], BF16) for i in range(2)]
                xeT = [sb(st, f"xeT{i}", [128, 8, CAP], BF16) for i in range(2)]
                hT = [sb(st, f"hT{i}", [128, 4, CAP], BF16) for i in range(2)]
                sg = [sb(st, f"sgm{i}", [128, CAP], F32) for i in range(2)]
                yst = [sb(st, f"yst{i}", [128, D], F32) for i in range(2)]
                lnp = sb(st, "lnp2", [128, 2, D], F32)
                y1 = [sb(st, f"y1_{i}", [128, D], F32) for i in range(2)]
                y2 = [sb(st, f"y2_{i}", [128, D], F32) for i in range(2)]
                xf = [sb(st, f"xf{i}", [128, D], F32) for i in range(2)]
                bst = [sb(st, f"bs2{i}", [128, 2, 6], F32) for i in range(2)]
                mv = [sb(st, f"mv2{i}", [128, 4], F32) for i in range(2)]
                bank = [0]

                def nb():
                    b = ps[bank[0] % 8]
                    bank[0] += 1
                    return b

                def load_e(e):
                    s = e % 2
                    DMA(P, EW[s][:, 0], wg_d[e].rearrange("(c p) n -> p c n", p=128), f"EWa{s}", eng="pool")
                    DMA(P, EW[s][:, 1], wu_d[e].rearrange("(c p) n -> p c n", p=128), f"EWb{s}", eng="pool")
                    DMA(P, EW[s][:, 2].rearrange("p (a b) n -> p a (b n)", b=2),
                        wd_d[e].rearrange("(c p) n -> p c n", p=128), f"EWc{s}", eng="pool")

                DMA(P, wr[:], wr_d.rearrange("(c p) n -> p c n", p=128), "wr", eng="pool")
                DMA(P, tri[:], tri_d, "tri", eng="pool")
                DMA(P, brt[:], br_d, "brt")
                DMA(P, ecoff[:], ecoff_d, "ecoff")
                DMA(P, lnp[:], lnp_d[:, 2048:4096].rearrange("p (a b) -> p a b", b=1024), "lnp2")
                MSET(P, onesb[:], 1.0)
                zt = sb(st, "zt", [128, 4096], BF16)
                MSET(P, zt[:], 0.0, eng="pool")
                for zi in range(16):
                    DMA(P, XS_s[zi * 512:(zi + 1) * 512, :].rearrange("(p a) d -> p (a d)", p=128), zt[:], "zt")
                load_e(0)
                load_e(1)
                for i in range(NBLK):
                    for hf in range(2):
                        b = nb()
                        bb = b[:, 0:256].bitcast(BF16)
                        for hh in range(4):
                            dc = hf * 4 + hh
                            TR(P, bb[:, hh * 128:(hh + 1) * 128], x1b[:, i, dc * 128:(dc + 1) * 128], ident[:])
                        CP(P, x1T[:, hf * 4:(hf + 1) * 4, i * 128:(i + 1) * 128],
                           bb.rearrange("p (h e) -> p h e", e=128), eng=("dve" if hf else "act"))
                for half in range(2):
                    b = nb()
                    for ii in range(8):
                        i = half * 8 + ii
                        for dc in range(8):
                            MM(P, b[:, ii * 36:(ii + 1) * 36], x1T[:, dc, i * 128:(i + 1) * 128], wr[:, dc, :],
                               start=(dc == 0), stop=(dc == 7))
                    TT(P, LG[:, half * 8:(half + 1) * 8, :], b[:, 0:288].rearrange("p (a b) -> p a b", b=36),
                       brt[:].unsqueeze(1).to_broadcast([128, 8, 36]), ALU.add)
                Gl = LG[:, :, 0:4]
                gmax, gsum, pg, m1, m2, dsh, ex, w1, w2, g1, g2, d1f = [sm[:, k, :] for k in range(12)]
                RED(P, gmax, Gl, ALU.max)
                TT(P, G4[:], Gl, gmax.unsqueeze(2).to_broadcast([128, 16, 4]), ALU.subtract)
                TT(P, gm[:], Gl, gmax.unsqueeze(2).to_broadcast([128, 16, 4]), ALU.is_equal)
                ACT(P, G4[:], G4[:], AF.Exp)
                RED(P, gsum, G4[:], ALU.add)
                RCP(P, pg, gsum)
                TS(P, gm[:], gm[:], BIG, ALU.mult, -BIG, ALU.add)
                TT(P, EM[:].rearrange("p a (g e) -> p a g e", e=8), LG[:, :, 4:36].rearrange("p a (g e) -> p a g e", e=8),
                   gm[:].unsqueeze(3).to_broadcast([128, 16, 4, 8]), ALU.add)
                RED(P, m1, EM[:], ALU.max)
                TT(P, mk1[:], EM[:], m1.unsqueeze(2).to_broadcast([128, 16, 32]), ALU.is_equal)
                STT(P, EM2[:].rearrange("p a b -> p (a b)"), mk1[:].rearrange("p a b -> p (a b)"), -BIG,
                    EM[:].rearrange("p a b -> p (a b)"), ALU.mult, ALU.add)
                RED(P, m2, EM2[:], ALU.max)
                TT(P, mk2[:], EM2[:], m2.unsqueeze(2).to_broadcast([128, 16, 32]), ALU.is_equal)
                TT(P, dsh, m2, m1, ALU.subtract)
                ACT(P, ex, dsh, AF.Exp)
                TS(P, w1, ex, 1.0, ALU.add)
                RCP(P, w1, w1)
                TT(P, w2, ex, w1, ALU.mult)
                TT(P, g1, pg, w1, ALU.mult)
                TT(P, g2, pg, w2, ALU.mult)
                TT(P, selb[:], mk1[:], mk2[:], ALU.add)
                bp = nb()
                for i in range(NBLK):
                    for j in range(i):
                        MM(P, bp[:, i * 32:(i + 1) * 32], onesb[:], selb[:, j, :], start=(j == 0), stop=False)
                    MM(P, bp[:, i * 32:(i + 1) * 32], tri[:], selb[:, i, :], start=(i == 0), stop=True)
                TT(P, slot[:], bp[:].rearrange("p (a b) -> p a b", b=32),
                   ecoff[:].unsqueeze(1).to_broadcast([128, 16, 32]), ALU.add)
                TT(P, tmp3[:], slot[:], mk1[:], ALU.mult)
                RED(P, d1f, tmp3[:], ALU.add)
                CP(P, di[:, 0, :], d1f)
                TT(P, tmp3[:], slot[:], mk2[:], ALU.mult)
                RED(P, d1f, tmp3[:], ALU.add)
                CP(P, di[:, 1, :], d1f)
                XS_all = XS_s[:, :]
                regc = {}

                def bnd(e):
                    if "r" not in regc:
                        regc["r"] = e.to_reg(NSLOT - 1)
                    return regc["r"]

                for i in range(NBLK):
                    for k in range(2):
                        idx = di[:, k, i:i + 1]
                        src = x1b[:, i, :]
                        P.add("pool", (lambda idx, src: (lambda e: e.indirect_dma_start(
                            out=XS_all, out_offset=bass.IndirectOffsetOnAxis(ap=idx, axis=0), in_=src, in_offset=None,
                            bounds_check=bnd(e), oob_is_err=False)))(idx, src),
                            [idx, src], [XS_all], dma_key="scat")
                DMA(P, xg[0][:], XS_s[0:CAP, :].rearrange("(a p) d -> p a d", p=128), "xg0")
                for e in range(NE):
                    s = e % 2
                    if e + 1 < NE:
                        DMA(P, xg[(e + 1) % 2][:], XS_s[(e + 1) * CAP:(e + 2) * CAP, :].rearrange("(a p) d -> p a d", p=128),
                            f"xg{(e + 1) % 2}")
                    for a in range(2):
                        for hf in range(2):
                            b = nb()
                            bb = b[:, 0:256].bitcast(BF16)
                            for hh in range(4):
                                dc = hf * 4 + hh
                                TR(P, bb[:, hh * 128:(hh + 1) * 128], xg[s][:, a, dc * 128:(dc + 1) * 128], ident[:])
                            CP(P, xeT[s][:, hf * 4:(hf + 1) * 4, a * 128:(a + 1) * 128],
                               bb.rearrange("p (h e) -> p h e", e=128), eng=("dve" if hf else "act"))
                    for fc in range(4):
                        bg = nb()
                        for dc in range(8):
                            MM(P, bg[:, 0:CAP], EW[s][:, 0, dc, fc * 128:(fc + 1) * 128], xeT[s][:, dc, :],
                               start=(dc == 0), stop=(dc == 7))
                        bu = nb()
                        for dc in range(8):
                            MM(P, bu[:, 0:CAP], EW[s][:, 1, dc, fc * 128:(fc + 1) * 128], xeT[s][:, dc, :],
                               start=(dc == 0), stop=(dc == 7))
                        ACT(P, sg[fc % 2][:], bg[:, 0:CAP], AF.Silu)
                        TT(P, hT[s][:, fc, :], sg[fc % 2][:], bu[:, 0:CAP], ALU.mult)
                    wdv = EW[s][:, 2].rearrange("p (a b) n -> p a (b n)", b=2)
                    for a in range(2):
                        Y = yst[a]
                        for hf in range(2):
                            b = nb()
                            for fc in range(4):
                                MM(P, b[:], hT[s][:, fc, a * 128:(a + 1) * 128], wdv[:, fc, hf * 512:(hf + 1) * 512],
                                   start=(fc == 0), stop=(fc == 3))
                            CP(P, Y[:, hf * 512:(hf + 1) * 512], b[:], eng=("dve" if hf else "act"))
                        DMA(P, YS_s[e * CAP + a * 128:e * CAP + (a + 1) * 128, :], Y[:], Y.name)
                    if e + 2 < NE:
                        load_e(e + 2)
                YS_all = YS_s[:, :]
                def fetch(i):
                    s = i % 2
                    for k, Yk in ((0, y1[s]), (1, y2[s])):
                        idx = di[:, k, i:i + 1]
                        dst = Yk[:, :]
                        P.add("pool", (lambda idx, dst: (lambda e: e.indirect_dma_start(
                            out=dst, out_offset=None, in_=YS_all, in_offset=bass.IndirectOffsetOnAxis(ap=idx, axis=0),
                            bounds_check=bnd(e), oob_is_err=False)))(idx, dst),
                            [idx, YS_all], [dst], dma_key=Yk.name)
                    DMA(P, xf[s][:], X1F_s[i * 128:(i + 1) * 128, :], xf[s].name)

                fetch(0)
                for i in range(NBLK):
                    s = i % 2
                    X = xf[s]
                    if i + 1 < NBLK:
                        fetch(i + 1)
                    TS(P, X[:], X[:], ALPHA, ALU.mult)
                    STT(P, X[:], y1[s][:], g1[:, i:i + 1], X[:], ALU.mult, ALU.add)
                    STT(P, X[:], y2[s][:], g2[:, i:i + 1], X[:], ALU.mult, ALU.add)
                    for hf in range(2):
                        P.add("dve", (lambda o, a: (lambda e: e.bn_stats(out=o, in_=a)))(bst[s][:, hf, :], X[:, hf * 512:(hf + 1) * 512]),
                              [X[:, hf * 512:(hf + 1) * 512]], [bst[s][:, hf, :]])
                    M = mv[s]
                    P.add("dve", (lambda o, a: (lambda e: e.bn_aggr(out=o, in_=a)))(M[:, 0:2], bst[s][:]),
                          [bst[s][:]], [M[:, 0:2]])
                    ACT(P, M[:, 2:3], M[:, 1:2], AF.Sqrt, bias=epsc[:, 0:1])
                    RCP(P, M[:, 3:4], M[:, 2:3])
                    TS(P, X[:], X[:], M[:, 0:1], ALU.subtract, M[:, 3:4], ALU.mult)
                    TT(P, X[:], X[:], lnp[:, 0, :], ALU.mult)
                    TT(P, X[:], X[:], lnp[:, 1, :], ALU.add)
                    DMA(P, y[i * 128:(i + 1) * 128, :], X[:], X.name)
                run_phase(g, P)
    return nc


def _bucket_table():
    dist = np.arange(0, 256, dtype=np.int32)
    max_exact = 16
    d = np.maximum(dist, 1).astype(np.float32)
    v = (np.log(d / np.float32(max_exact)) / np.float32(math.log(128 / max_exact)) * np.float32(32 - max_exact))
    large = max_exact + v.astype(np.float32).astype(np.int32)
    large = np.minimum(large, 31)
    return np.where(dist < max_exact, dist, large)


def _col(v, n):
    return np.ascontiguousarray(np.asarray(v, np.float32).reshape(n, 128).T)


def prepare_inputs(x, w_in, b_in, diff_lambda, head_norm_g, w_o_attn, rel_bias, conv_w, conv_b,
                   conv_ln_g, conv_ln_b, w_conv_out, b_conv_out, w_out, ln1_g, ln1_b,
                   router_g_w, router_g_b, router_e_w, router_e_b, expert_w_gate, expert_w_up,
                   expert_w_down, ln2_g, ln2_b):
    f = lambda a: np.asarray(a, np.float32)
    x = f(x)
    w_in0 = np.ascontiguousarray(f(w_in)[0])
    b_in0 = f(b_in)[0]
    rel = f(rel_bias)
    bucket = _bucket_table()
    kk = np.arange(128)[:, None]
    qq = np.arange(128)[None, :]
    d0 = qq - kk
    tile_diag = np.where(d0[None] >= 0, rel[bucket[np.maximum(d0, 0)]].transpose(2, 0, 1), np.float32(-1e30))
    tile_prev = rel[bucket[d0 + 128]].transpose(2, 0, 1)
    tile_far = np.broadcast_to(rel[31][:, None, None], (8, 128, 128))
    tile_mask = np.full((8, 128, 128), -1e30, np.float32)
    shared = {
        "w_in": w_in0,
        "bcol": _col(b_in0, 56),
        "bv_bc": np.ascontiguousarray(np.broadcast_to(b_in0[2048:3072], (128, 1024))),
        "lam_bc": np.ascontiguousarray(np.broadcast_to(f(diff_lambda)[0].reshape(256), (128, 256))),
        "hng_bc": np.ascontiguousarray(np.broadcast_to(f(head_norm_g)[0], (128, 128))),
        "relc": np.ascontiguousarray(np.broadcast_to(rel[31], (128, 8))),
        "ident": np.eye(128, dtype=np.float32),
        "tri": np.triu(np.ones((128, 128), np.float32), 1),
        "ecoff": np.ascontiguousarray(np.broadcast_to((np.arange(32) * CAP).astype(np.float32), (128, 32))),
        "convw": np.ascontiguousarray(f(conv_w)[0].reshape(31, 8, 128).transpose(2, 1, 0).reshape(128, 8 * 31)),
        "cvec": np.ascontiguousarray(np.concatenate([_col(f(conv_b)[0], 8), _col(f(conv_ln_g)[0], 8),
                                                     _col(f(conv_ln_b)[0], 8), _col(f(b_conv_out)[0], 8)], axis=1)),
        "w_o_attn": np.ascontiguousarray(f(w_o_attn)[0]),
        "w_conv_out": np.ascontiguousarray(f(w_conv_out)[0]),
        "w_out": np.ascontiguousarray(f(w_out)[0]),
        "lnp": np.ascontiguousarray(np.concatenate([np.broadcast_to(f(v)[0], (128, 1024))
                                                    for v in (ln1_g, ln1_b, ln2_g, ln2_b)], axis=1)),
        "w_r": np.ascontiguousarray(np.concatenate([f(router_g_w)[0], f(router_e_w)[0]], axis=1)),
        "b_r": np.ascontiguousarray(np.broadcast_to(np.concatenate([f(router_g_b)[0], f(router_e_b)[0]]), (128, 36))),
        "e_wg": np.ascontiguousarray(f(expert_w_gate)[0]),
        "e_wu": np.ascontiguousarray(f(expert_w_up)[0]),
        "e_wd": np.ascontiguousarray(f(expert_w_down)[0]),
    }
    in_maps = []
    for c in range(8):
        b, p = c // 2, c % 2
        blocks = x[b].reshape(32, 128, D)
        order = [2 * i + p for i in range(16)] + [2 * i + (1 - p) for i in range(16)]
        x_loc = blocks[order].reshape(S, D)
        halo = np.zeros((16, 32, D), np.float32)
        for i in range(16):
            start = (2 * i + p) * 128
            if start >= 32:
                halo[i] = x[b, start - 32:start]
        if p == 1:
            tiles = np.stack([tile_diag, tile_prev, tile_far], axis=1)
        else:
            tiles = np.stack([tile_diag, tile_mask, tile_prev], axis=1)
        m = dict(shared)
        m["xT"] = np.ascontiguousarray(x_loc.T)
        m["xhT"] = np.ascontiguousarray(halo.reshape(512, D).T)
        m["xtok"] = np.ascontiguousarray(x_loc[:TOWN])
        m["hm"] = np.full((128, 1), float(p), np.float32)
        m["biasT"] = np.ascontiguousarray(tiles.transpose(2, 0, 1, 3).reshape(128, 8 * 3 * 128).astype(np.float32))
        in_maps.append(m)
    return in_maps


def kernel(**inputs):
    in_maps = prepare_inputs(**inputs)
    nc = build_nc()
    res = run_bass_kernel_spmd(nc, in_maps, core_ids=list(range(8)))
    out = np.zeros((4, 32, 128, D), np.float32)
    for c in range(8):
        b, p = c // 2, c % 2
        yc = np.asarray(res.results[c]["y"]).reshape(16, 128, D)
        for i in range(16):
            out[b, 2 * i + p] = yc[i]
    return out.reshape(4, S, D)
```
